# Optimizing a Trainium2 kernel written in Bass

```python
import math
import jax, jax.numpy as jnp
from jax import lax
import numpy as np

D_MODEL = 1024
BATCH = 2
SEQ = 16384
DEPTH = 4

N_MIXERS = 3
N_MLSTM = (DEPTH + 2) // 3
N_S5 = (DEPTH + 1) // 3
N_SWA = DEPTH // 3
DN_ALPHA = (2 * DEPTH) ** 0.25
DN_BETA = (8 * DEPTH) ** -0.25
LN_EPS = 1e-5

M_HEADS = 4
M_DQK = D_MODEL // 8
M_DV = D_MODEL // M_HEADS
M_CONV = 4
M_CHUNK = 64
M_IN = 2 * M_HEADS * M_DQK + M_HEADS * M_DV + D_MODEL + 2 * M_HEADS

S5_GROUP = 16
S5_GROUPS = D_MODEL // S5_GROUP
S5_STATE = 64
S5_CHUNK = 128

A_HEADS = 16
A_KV_HEADS = 4
A_GROUP = A_HEADS // A_KV_HEADS
A_HEAD_DIM = D_MODEL // A_HEADS
WINDOW = 128
A_BLOCK = 128

E_GROUPS = 4
E_PER_GROUP = 8
N_EXPERTS = E_GROUPS * E_PER_GROUP
TOP_K = 2
D_EXPERT = 512
MOE_BLOCK = 128

kernel_name = 'hybrid_mlstm_s5_swa_hmoe_deepnorm'


def _layer_norm(x, g, b):
    xf = x.astype(jnp.float32)
    mu = xf.mean(-1, keepdims=True)
    var = jnp.mean(jnp.square(xf - mu), -1, keepdims=True)
    return ((xf - mu) * lax.rsqrt(var + LN_EPS)).astype(x.dtype) * g + b


def _causal_dwconv(x, w, b):
    k = w.shape[0]
    y = lax.conv_general_dilated(x, w[:, None, :], window_strides=(1,), padding=[(k - 1, 0)],
                                 dimension_numbers=('NWC', 'WIO', 'NWC'), feature_group_count=x.shape[-1])
    return y + b


def _mlstm_chunk_step(carry, inp):
    C, n, m = carry
    q, k, v, li, lf = inp
    L = q.shape[2]
    causal = jnp.tril(jnp.ones((L, L), bool))
    b = jnp.cumsum(lf, axis=-1)
    d = jnp.where(causal, b[..., :, None] - b[..., None, :] + li[..., None, :], -jnp.inf)
    inter = b + m[..., None]
    m_t = jnp.maximum(inter, d.max(-1))
    w = jnp.exp(d - m_t[..., None]) * jnp.einsum('bhtk,bhsk->bhts', q, k)
    e_inter = jnp.exp(inter - m_t)
    num = jnp.einsum('bhts,bhsv->bhtv', w, v) + e_inter[..., None] * jnp.einsum('bhtk,bhkv->bhtv', q, C)
    den = w.sum(-1) + e_inter * jnp.einsum('bhtk,bhk->bht', q, n)
    h = num / jnp.maximum(jnp.abs(den), jnp.exp(-m_t))[..., None]
    b_last = b[..., -1]
    g = b_last[..., None] - b + li
    m_new = jnp.maximum(b_last + m, g.max(-1))
    decay = jnp.exp(b_last + m - m_new)
    wk = jnp.exp(g - m_new[..., None])[..., None] * k
    C_new = decay[..., None, None] * C + jnp.einsum('bhsk,bhsv->bhkv', wk, v)
    n_new = decay[..., None] * n + wk.sum(2)
    return (C_new, n_new, m_new), h


def _mlstm_mixer(h, w_in, conv_w, conv_b, b_if, norm_g, w_out):
    Bn, S, _ = h.shape
    nc = S // M_CHUNK
    f32 = jnp.float32
    n_qk = 2 * M_HEADS * M_DQK
    n_v = M_HEADS * M_DV
    proj = h @ w_in
    qk = jax.nn.silu(_causal_dwconv(proj[..., :n_qk], conv_w, conv_b))
    v = proj[..., n_qk:n_qk + n_v]
    o = proj[..., n_qk + n_v:n_qk + n_v + D_MODEL]
    gates = (proj[..., n_qk + n_v + D_MODEL:] + b_if).astype(f32)
    li = gates[..., :M_HEADS]
    lf = jax.nn.log_sigmoid(gates[..., M_HEADS:])

    def to_chunks(t, dh):
        return t.astype(f32).reshape(Bn, nc, M_CHUNK, M_HEADS, dh).transpose(1, 0, 3, 2, 4)

    def gate_chunks(t):
        return t.reshape(Bn, nc, M_CHUNK, M_HEADS).transpose(1, 0, 3, 2)

    q = to_chunks(qk[..., :M_HEADS * M_DQK], M_DQK)
    k = to_chunks(qk[..., M_HEADS * M_DQK:], M_DQK) * (M_DQK ** -0.5)
    vc = to_chunks(v, M_DV)
    init = (jnp.zeros((Bn, M_HEADS, M_DQK, M_DV), f32), jnp.zeros((Bn, M_HEADS, M_DQK), f32),
            jnp.zeros((Bn, M_HEADS), f32))
    _, hs = lax.scan(_mlstm_chunk_step, init, (q, k, vc, gate_chunks(li), gate_chunks(lf)))
    hs = hs.transpose(1, 0, 3, 2, 4).reshape(Bn, S, M_HEADS, M_DV)
    mu = hs.mean(-1, keepdims=True)
    var = jnp.mean(jnp.square(hs - mu), -1, keepdims=True)
    hn = ((hs - mu) * lax.rsqrt(var + LN_EPS)).reshape(Bn, S, n_v).astype(h.dtype) * norm_g
    return (hn * jax.nn.sigmoid(o)) @ w_out


def _cmul_scan_op(e1, e2):
    a1r, a1i, b1r, b1i = e1
    a2r, a2i, b2r, b2i = e2
    return (a2r * a1r - a2i * a1i, a2r * a1i + a2i * a1r,
            a2r * b1r - a2i * b1i + b2r, a2r * b1i + a2i * b1r + b2i)


def _s5_mixer(h, w_in, lam_re, lam_im, log_dt, b_re, b_im, c_re, c_im, d_skip, w_glu):
    Bn, S, _ = h.shape
    f32 = jnp.float32
    L = S5_CHUNK
    nc = S // L
    u = h @ w_in
    lr = lam_re.astype(f32)
    lim = lam_im.astype(f32)
    dt = jnp.exp(log_dt.astype(f32))[:, None]
    mag = jnp.exp(lr * dt)
    abar_re = mag * jnp.cos(lim * dt)
    abar_im = mag * jnp.sin(lim * dt)
    lam_sq = lr * lr + lim * lim
    t_re = ((abar_re - 1.0) * lr + abar_im * lim) / lam_sq
    t_im = (abar_im * lr - (abar_re - 1.0) * lim) / lam_sq
    br = b_re.astype(f32)
    bi = b_im.astype(f32)
    bb_re = t_re[..., None] * br - t_im[..., None] * bi
    bb_im = t_re[..., None] * bi + t_im[..., None] * br
    steps = jnp.arange(1, L + 1, dtype=f32)[:, None, None]
    pmag = jnp.exp(lr * dt * steps)
    pw_re = pmag * jnp.cos(lim * dt * steps)
    pw_im = pmag * jnp.sin(lim * dt * steps)
    cr = c_re.astype(f32)
    ci = c_im.astype(f32)

    def step(carry, u_c):
        x_re, x_im = carry
        bu_re = jnp.einsum('blgi,gpi->blgp', u_c, bb_re)
        bu_im = jnp.einsum('blgi,gpi->blgp', u_c, bb_im)
        a_re = jnp.broadcast_to(abar_re, bu_re.shape)
        a_im = jnp.broadcast_to(abar_im, bu_re.shape)
        _, _, xs_re, xs_im = lax.associative_scan(_cmul_scan_op, (a_re, a_im, bu_re, bu_im), axis=1)
        xs_re = xs_re + pw_re * x_re[:, None] - pw_im * x_im[:, None]
        xs_im = xs_im + pw_re * x_im[:, None] + pw_im * x_re[:, None]
        y = jnp.einsum('blgp,gip->blgi', xs_re, cr) - jnp.einsum('blgp,gip->blgi', xs_im, ci)
        return (xs_re[:, -1], xs_im[:, -1]), y

    uc = u.astype(f32).reshape(Bn, nc, L, S5_GROUPS, S5_GROUP).transpose(1, 0, 2, 3, 4)
    init = (jnp.zeros((Bn, S5_GROUPS, S5_STATE), f32), jnp.zeros((Bn, S5_GROUPS, S5_STATE), f32))
    _, ys = lax.scan(step, init, uc)
    y = ys.transpose(1, 0, 2, 3, 4).reshape(Bn, S, D_MODEL).astype(h.dtype) + d_skip * u
    z = jax.nn.gelu(y) @ w_glu
    return z[..., :D_MODEL] * jax.nn.sigmoid(z[..., D_MODEL:])


def _swa_mixer(h, w_qkv, b_qkv, sinks, w_o):
    Bn, S, _ = h.shape
    nb = S // A_BLOCK
    f32 = jnp.float32
    qkv = h @ w_qkv + b_qkv
    nq = A_HEADS * A_HEAD_DIM
    nkv = A_KV_HEADS * A_HEAD_DIM
    q = qkv[..., :nq].reshape(Bn, nb, A_BLOCK, A_KV_HEADS, A_GROUP, A_HEAD_DIM)
    k = qkv[..., nq:nq + nkv].reshape(Bn, S, A_KV_HEADS, A_HEAD_DIM)
    v = qkv[..., nq + nkv:].reshape(Bn, S, A_KV_HEADS, A_HEAD_DIM)

    def band(t):
        tp = jnp.pad(t, ((0, 0), (A_BLOCK, 0), (0, 0), (0, 0))).reshape(Bn, nb + 1, A_BLOCK, A_KV_HEADS, A_HEAD_DIM)
        return jnp.concatenate([tp[:, :-1], tp[:, 1:]], axis=2)

    kw = band(k)
    vw = band(v)
    s = jnp.einsum('bnqhgd,bnkhd->bhgnqk', q.astype(f32), kw.astype(f32)) * (A_HEAD_DIM ** -0.5)
    blk = jnp.arange(nb)[:, None, None] * A_BLOCK
    qpos = blk + jnp.arange(A_BLOCK)[None, :, None]
    kpos = blk - A_BLOCK + jnp.arange(2 * A_BLOCK)[None, None, :]
    valid = (kpos <= qpos) & (qpos - kpos < WINDOW) & (kpos >= 0)
    s = jnp.where(valid, s, -jnp.inf)
    sink = sinks.astype(f32).reshape(A_KV_HEADS, A_GROUP)[None, :, :, None, None]
    m = jnp.maximum(s.max(-1), sink)
    p = jnp.exp(s - m[..., None])
    p = p / (p.sum(-1) + jnp.exp(sink - m))[..., None]
    o = jnp.einsum('bhgnqk,bnkhd->bnqhgd', p.astype(h.dtype), vw).reshape(Bn, S, D_MODEL)
    return o @ w_o


def _hier_moe(h, w_group, b_group, w_router, b_router, w1, w3, w2):
    Bn, S, Dm = h.shape
    T = Bn * S
    f32 = jnp.float32
    xt = h.reshape(T, Dm)
    glog = (xt @ w_group + b_group).astype(f32)
    g_sel = jnp.argmax(glog, -1)
    p_g = jnp.take_along_axis(jax.nn.softmax(glog, -1), g_sel[:, None], -1)[:, 0]
    elog = (xt @ w_router + b_router).astype(f32).reshape(T, E_GROUPS, E_PER_GROUP)
    elog_g = jnp.take_along_axis(elog, g_sel[:, None, None], 1)[:, 0]
    top_v, top_i = lax.top_k(elog_g, TOP_K)
    w_tok = jax.nn.softmax(top_v, -1) * p_g[:, None]
    e_a = (g_sel[:, None] * E_PER_GROUP + top_i).reshape(-1)
    tok_a = jnp.repeat(jnp.arange(T, dtype=jnp.int32), TOP_K)
    w_a = w_tok.reshape(-1)
    n_assign = T * TOP_K
    order = jnp.argsort(e_a)
    se = e_a[order]
    stok = tok_a[order]
    sw = w_a[order]
    counts = jax.ops.segment_sum(jnp.ones_like(e_a), e_a, num_segments=N_EXPERTS)
    padded = (counts + MOE_BLOCK - 1) // MOE_BLOCK * MOE_BLOCK
    start = jnp.cumsum(counts) - counts
    pend = jnp.cumsum(padded)
    pstart = pend - padded
    pos = pstart[se] + (jnp.arange(n_assign) - start[se])
    P = n_assign + N_EXPERTS * MOE_BLOCK
    nblk = P // MOE_BLOCK
    buf_tok = jnp.zeros((P,), jnp.int32).at[pos].set(stok)
    buf_w = jnp.zeros((P,), f32).at[pos].set(sw)
    buf_x = jnp.zeros((P, Dm), h.dtype).at[pos].set(xt[stok])
    blk_e = jnp.clip(jnp.searchsorted(pend, jnp.arange(nblk) * MOE_BLOCK, side='right'), 0, N_EXPERTS - 1)

    def expert_block(args):
        xb, e = args
        return (jax.nn.silu(xb @ w1[e]) * (xb @ w3[e])) @ w2[e]

    y = lax.map(expert_block, (buf_x.reshape(nblk, MOE_BLOCK, Dm), blk_e)).reshape(P, Dm)
    y = y * buf_w[:, None].astype(y.dtype)
    return jnp.zeros((T, Dm), h.dtype).at[buf_tok].add(y).reshape(Bn, S, Dm)


def setup_inputs(seed: int = 0) -> dict:
    key = jax.random.key(seed)
    ks = iter(jax.random.split(key, 64))
    f32 = jnp.float32
    D = D_MODEL

    def nrm(shape, std):
        return jax.random.normal(next(ks), shape, f32) * std

    inp = {}
    inp['x'] = nrm((BATCH, SEQ, D), 1.0)
    inp['c'] = nrm((BATCH, D), 1.0)
    inp['ada_w'] = nrm((DEPTH, D, 6 * D), 0.5 * D ** -0.5)
    inp['ada_b'] = nrm((DEPTH, 6 * D), 0.01)
    inp['ln1_g'] = 1.0 + nrm((DEPTH, D), 0.02)
    inp['ln1_b'] = nrm((DEPTH, D), 0.01)
    inp['ln2_g'] = 1.0 + nrm((DEPTH, D), 0.02)
    inp['ln2_b'] = nrm((DEPTH, D), 0.01)
    inp['mlstm_w_in'] = nrm((N_MLSTM, D, M_IN), D ** -0.5)
    inp['mlstm_conv_w'] = nrm((N_MLSTM, M_CONV, 2 * M_HEADS * M_DQK), M_CONV ** -0.5)
    inp['mlstm_conv_b'] = nrm((N_MLSTM, 2 * M_HEADS * M_DQK), 0.01)
    f_bias = 3.0 + 3.0 * jax.random.uniform(next(ks), (N_MLSTM, M_HEADS), f32)
    inp['mlstm_b_if'] = jnp.concatenate([nrm((N_MLSTM, M_HEADS), 0.1) - 1.0, f_bias], -1)
    inp['mlstm_norm_g'] = 1.0 + nrm((N_MLSTM, M_HEADS * M_DV), 0.02)
    inp['mlstm_w_out'] = nrm((N_MLSTM, M_HEADS * M_DV, D), (M_HEADS * M_DV) ** -0.5 * DN_BETA)
    inp['s5_w_in'] = nrm((N_S5, D, D), D ** -0.5)
    inp['s5_lam_re'] = -0.5 + nrm((N_S5, S5_GROUPS, S5_STATE), 0.01)
    inp['s5_lam_im'] = math.pi * jnp.arange(S5_STATE, dtype=f32)[None, None, :] + nrm((N_S5, S5_GROUPS, S5_STATE), 0.01)
    inp['s5_log_dt'] = jax.random.uniform(next(ks), (N_S5, S5_GROUPS), f32, minval=math.log(1e-3), maxval=math.log(1e-1))
    inp['s5_b_re'] = nrm((N_S5, S5_GROUPS, S5_STATE, S5_GROUP), (2 * S5_GROUP) ** -0.5)
    inp['s5_b_im'] = nrm((N_S5, S5_GROUPS, S5_STATE, S5_GROUP), (2 * S5_GROUP) ** -0.5)
    inp['s5_c_re'] = nrm((N_S5, S5_GROUPS, S5_GROUP, S5_STATE), S5_STATE ** -0.5)
    inp['s5_c_im'] = nrm((N_S5, S5_GROUPS, S5_GROUP, S5_STATE), S5_STATE ** -0.5)
    inp['s5_d'] = nrm((N_S5, D), 1.0)
    inp['s5_w_glu'] = jnp.concatenate([nrm((N_S5, D, D), D ** -0.5 * DN_BETA), nrm((N_S5, D, D), D ** -0.5)], -1)
    n_qkv = (A_HEADS + 2 * A_KV_HEADS) * A_HEAD_DIM
    inp['swa_w_qkv'] = nrm((N_SWA, D, n_qkv), D ** -0.5)
    inp['swa_b_qkv'] = nrm((N_SWA, n_qkv), 0.01)
    inp['swa_sinks'] = nrm((N_SWA, A_HEADS), 0.5)
    inp['swa_w_o'] = nrm((N_SWA, A_HEADS * A_HEAD_DIM, D), (A_HEADS * A_HEAD_DIM) ** -0.5 * DN_BETA)
    inp['moe_w_group'] = nrm((DEPTH, D, E_GROUPS), D ** -0.5)
    inp['moe_b_group'] = nrm((DEPTH, E_GROUPS), 0.01)
    inp['moe_w_router'] = nrm((DEPTH, D, N_EXPERTS), D ** -0.5)
    inp['moe_b_router'] = nrm((DEPTH, N_EXPERTS), 0.01)
    inp['moe_w1'] = nrm((DEPTH, N_EXPERTS, D, D_EXPERT), D ** -0.5)
    inp['moe_w3'] = nrm((DEPTH, N_EXPERTS, D, D_EXPERT), D ** -0.5)
    inp['moe_w2'] = nrm((DEPTH, N_EXPERTS, D_EXPERT, D), D_EXPERT ** -0.5 * DN_BETA)
    return inp


def reference(x, c, ada_w, ada_b, ln1_g, ln1_b, ln2_g, ln2_b,
              mlstm_w_in, mlstm_conv_w, mlstm_conv_b, mlstm_b_if, mlstm_norm_g, mlstm_w_out,
              s5_w_in, s5_lam_re, s5_lam_im, s5_log_dt, s5_b_re, s5_b_im, s5_c_re, s5_c_im, s5_d, s5_w_glu,
              swa_w_qkv, swa_b_qkv, swa_sinks, swa_w_o,
              moe_w_group, moe_b_group, moe_w_router, moe_b_router, moe_w1, moe_w3, moe_w2):
    cond = jax.nn.silu(c)
    for i in range(DEPTH):
        mod = cond @ ada_w[i] + ada_b[i]
        sh1, sc1, g1, sh2, sc2, g2 = jnp.split(mod, 6, axis=-1)
        hin = x * (1.0 + sc1[:, None]) + sh1[:, None]
        kind = i % N_MIXERS
        j = i // N_MIXERS
        if kind == 0:
            y = _mlstm_mixer(hin, mlstm_w_in[j], mlstm_conv_w[j], mlstm_conv_b[j], mlstm_b_if[j],
                             mlstm_norm_g[j], mlstm_w_out[j])
        elif kind == 1:
            y = _s5_mixer(hin, s5_w_in[j], s5_lam_re[j], s5_lam_im[j], s5_log_dt[j], s5_b_re[j], s5_b_im[j],
                          s5_c_re[j], s5_c_im[j], s5_d[j], s5_w_glu[j])
        else:
            y = _swa_mixer(hin, swa_w_qkv[j], swa_b_qkv[j], swa_sinks[j], swa_w_o[j])
        x = _layer_norm(DN_ALPHA * x + g1[:, None] * y, ln1_g[i], ln1_b[i])
        hin = x * (1.0 + sc2[:, None]) + sh2[:, None]
        y = _hier_moe(hin, moe_w_group[i], moe_b_group[i], moe_w_router[i], moe_b_router[i],
                      moe_w1[i], moe_w3[i], moe_w2[i])
        x = _layer_norm(DN_ALPHA * x + g2[:, None] * y, ln2_g[i], ln2_b[i])
    return x
```

```python
import math
import itertools
from contextlib import ExitStack
import numpy as np
import concourse.bass as bass
import concourse.mybir as mybir
from concourse.bass_utils import run_bass_kernel_spmd

F32 = mybir.dt.float32
BF16 = mybir.dt.bfloat16
I32 = mybir.dt.int32
AF = mybir.ActivationFunctionType
ALU = mybir.AluOpType
AX = mybir.AxisListType

D = 1024
DEPTH = 4
ALPHA = (2 * DEPTH) ** 0.25
EPS = 1e-5
NE = 32
DE = 512
QSCALE = 128 ** -0.5
ENG = ['pe', 'dve', 'act', 'pool', 'sp']
NDS = 8
NDSQ = {'sp': 8, 'pool': 6, 'act': 8}
KNOBS = {}


class Em:
    def __init__(self, nc, es):
        self.nc = nc
        self.sem = {}
        for e in ENG:
            self.sem[e] = es.enter_context(nc.semaphore('s_' + e))
        for q in ('sp', 'pool', 'act'):
            for i in range(NDS):
                self.sem[('d', q, i)] = es.enter_context(nc.semaphore('d_%s%d' % (q, i)))
        self.cnt = {e: 0 for e in ENG}
        self.dnext = {'sp': 0, 'pool': 0, 'act': 0}
        self.seen = {e: {} for e in ENG}
        self.stream = {e: [] for e in ENG}
        self.res = {}

    def _deps(self, reads, writes):
        toks = []
        for r in reads:
            st = self.res.get(r)
            if st is not None and st[0] is not None:
                toks.append(st[0])
        for w in writes:
            st = self.res.get(w)
            if st is not None:
                if st[0] is not None:
                    toks.append(st[0])
                toks.extend(st[1].items())
        return toks

    def _mark(self, tok, reads, writes):
        for r in reads:
            st = self.res.get(r)
            if st is None:
                st = [None, {}]
                self.res[r] = st
            if st[1].get(tok[0], 0) < tok[1]:
                st[1][tok[0]] = tok[1]
        for w in writes:
            self.res[w] = [tok, {}]

    def _waits(self, eng, toks):
        need = {}
        for k, v in toks:
            if k == 'pe' and eng == 'pe':
                continue
            if need.get(k, 0) < v:
                need[k] = v
        out = []
        seen = self.seen[eng]
        for k, v in need.items():
            if seen.get(k, 0) < v:
                seen[k] = v
                out.append((k, v))
        return out

    def op(self, eng, fn, r=(), w=()):
        toks = self._deps(r, w)
        waits = self._waits(eng, toks)
        self.cnt[eng] += 1
        tok = (eng, self.cnt[eng])
        self._mark(tok, r, w)
        self.stream[eng].append((waits, fn, eng, 1))

    def dma(self, q, fn, r=(), w=()):
        i = self.dnext[q]
        self.dnext[q] += 1
        nq = NDSQ[q]
        key = ('d', q, i % nq)
        val = 16 * (i // nq + 1)
        toks = self._deps(r, w)
        if i >= nq:
            toks.append((key, val - 16))
        waits = self._waits(q, toks)
        tok = (key, val)
        self._mark(tok, r, w)
        self.stream[q].append((waits, fn, key, 16))

    def fence(self):
        waits = []
        for e in ENG:
            if self.cnt[e] > 0:
                waits.append((e, self.cnt[e]))
        for q in ('sp', 'pool', 'act'):
            n = self.dnext[q]
            for sl in range(min(n, NDSQ[q])):
                uses = (n - 1 - sl) // NDSQ[q] + 1
                waits.append((('d', q, sl), 16 * uses))
        for e in ENG:
            ww = self._waits(e, list(waits))
            if ww:
                self.stream[e].append((ww, None, None, 0))

    def finish(self):
        waits = []
        for e in ENG:
            if self.cnt[e] > 0:
                waits.append((e, self.cnt[e]))
        for q in ('sp', 'pool', 'act'):
            n = self.dnext[q]
            for sl in range(min(n, NDSQ[q])):
                uses = (n - 1 - sl) // NDSQ[q] + 1
                waits.append((('d', q, sl), 16 * uses))
        self.stream['sp'].append((waits, None, None, 0))

    def emit(self, block):
        def run(name):
            def body(e):
                for waits, fn, key, inc in self.stream[name]:
                    for k, v in waits:
                        e.wait_ge(self.sem[k], v)
                    if fn is not None:
                        ins = fn(e)
                        ins.then_inc(self.sem[key], inc)
            return body
        block.tensor(run('pe'))
        block.vector(run('dve'))
        block.scalar(run('act'))
        block.gpsimd(run('pool'))
        block.sync(run('sp'))

    def tt(self, eng, out, a, b, op, r, w):
        self.op(eng, lambda e: e.tensor_tensor(out=out, in0=a, in1=b, op=op), r, w)

    def ts(self, eng, out, a, s1, op0, r, w, s2=None, op1=None):
        if op1 is None:
            self.op(eng, lambda e: e.tensor_scalar(out=out, in0=a, scalar1=s1, scalar2=None, op0=op0), r, w)
        else:
            self.op(eng, lambda e: e.tensor_scalar(out=out, in0=a, scalar1=s1, scalar2=s2, op0=op0, op1=op1), r, w)

    def stt(self, out, a, sc, b, op0, op1, r, w):
        self.op('dve', lambda e: e.scalar_tensor_tensor(out=out, in0=a, scalar=sc, in1=b, op0=op0, op1=op1), r, w)

    def act(self, out, a, func, r, w, bias=None, scale=None, accum=None):
        kw = {}
        if bias is not None:
            kw['bias'] = bias
        if scale is not None:
            kw['scale'] = scale
        if accum is not None:
            kw['accum_out'] = accum
        self.op('act', lambda e: e.activation(out=out, in_=a, func=func, **kw), r, w)

    def mm(self, out, lhsT, rhs, start, stop, r, w):
        self.op('pe', lambda e: e.matmul(out, lhsT=lhsT, rhs=rhs, start=start, stop=stop), r, w)

    def tr(self, out, a, ident, r, w):
        self.op('pe', lambda e: e.transpose(out=out, in_=a, identity=ident), r, w)

    def cp(self, eng, out, a, r, w):
        if eng == 'act':
            self.op('act', lambda e: e.activation(out=out, in_=a, func=AF.Copy), r, w)
        else:
            self.op(eng, lambda e: e.tensor_copy(out=out, in_=a), r, w)

    def red(self, out, a, op, r, w):
        self.op('dve', lambda e: e.tensor_reduce(out=out, in_=a, axis=AX.X, op=op), r, w)

    def memset(self, eng, ap, val, w):
        self.op(eng, lambda e: e.memset(ap, val), (), w)

    def load(self, q, out, in_, r, w):
        self.dma(q, lambda e: e.dma_start(out=out, in_=in_), r, w)


def build(S, layers, ncores=2, dbg=False):
    NT = S // 128
    BK = 256
    SUB = BK // 128
    NB = (2 * S + NE * (BK - 1) + BK - 1) // BK
    PR = NB * BK
    nc = bass.Bass("TRN2", target_bir_lowering=False)
    es = ExitStack()

    def din(name, shape, dt=F32):
        return nc.dram_tensor(name, list(shape), dt, kind="ExternalInput").ap()

    def dint(name, shape, dt=F32):
        return nc.dram_tensor(name, list(shape), dt, kind="Internal").ap()

    x_in = din("x", [S, D])
    c_in = din("c", [1, D])
    ada_w = din("ada_w", [DEPTH, D, 6 * D]); ada_b = din("ada_b", [DEPTH, 6 * D])
    ln1_g = din("ln1_g", [DEPTH, D]); ln1_b = din("ln1_b", [DEPTH, D])
    ln2_g = din("ln2_g", [DEPTH, D]); ln2_b = din("ln2_b", [DEPTH, D])
    m_w_in = din("mlstm_w_in", [2, D, 3080]); m_conv_w = din("mlstm_conv_w", [2, 4, 1024])
    m_conv_b = din("mlstm_conv_b", [2, 1024]); m_b_if = din("mlstm_b_if", [2, 8])
    m_norm_g = din("mlstm_norm_g", [2, 1024]); m_w_out = din("mlstm_w_out", [2, 1024, D])
    s_w_in = din("s5_w_in", [1, D, D]); s_lam_re = din("s5_lam_re", [1, 64, 64]); s_lam_im = din("s5_lam_im", [1, 64, 64])
    s_log_dt = din("s5_log_dt", [1, 64]); s_b_re = din("s5_b_re", [1, 64, 64, 16]); s_b_im = din("s5_b_im", [1, 64, 64, 16])
    s_c_re = din("s5_c_re", [1, 64, 16, 64]); s_c_im = din("s5_c_im", [1, 64, 16, 64])
    s_d = din("s5_d", [1, D]); s_w_glu = din("s5_w_glu", [1, D, 2 * D])
    a_w_qkv = din("swa_w_qkv", [1, D, 1536]); a_b_qkv = din("swa_b_qkv", [1, 1536])
    a_sinks = din("swa_sinks", [1, 16]); a_w_o = din("swa_w_o", [1, 1024, D])
    e_w_group = din("moe_w_group", [DEPTH, D, 4]); e_b_group = din("moe_b_group", [DEPTH, 4])
    e_w_router = din("moe_w_router", [DEPTH, D, NE]); e_b_router = din("moe_b_router", [DEPTH, NE])
    e_w1 = din("moe_w1", [DEPTH * NE * 256, 2048]); e_w3 = din("moe_w3", [DEPTH * NE * 256, 2048]); e_w2 = din("moe_w2", [DEPTH * NE * 256, 2048])
    out_d = nc.dram_tensor("out", [S, D], F32, kind="ExternalOutput").ap()
    dbg_d = nc.dram_tensor("dbg", [S, D], F32, kind="ExternalOutput").ap() if dbg else None

    modv = dint("modv", [DEPTH, 6 * D])
    x1d = dint("x1d", [S, D])
    h2d = dint("h2d", [S, D], BF16)
    xsd = dint("xsd", [PR, D], BF16)
    ysd = dint("ysd", [PR, D])

    em = Em(nc, es)
    uid = itertools.count()
    regcache = {}

    def sb(name, shape, dt=F32):
        return es.enter_context(nc.sbuf_tensor(name, list(shape), dt))

    def ps(name, shape, dt=F32):
        return es.enter_context(nc.psum_tensor(name, list(shape), dt))

    pb = [ps("pb%d" % i, [128, 512]) for i in range(8)]

    iot = sb("iot", [128, 128], I32)
    ident_f = sb("ident_f", [128, 128]); ident_b = sb("ident_b", [128, 128], BF16)
    tstrict = sb("tstrict", [128, 128], BF16); ones_b = sb("ones_b", [128, 128], BF16)
    mmask = sb("mmask", [128, 128])
    sel = sb("sel", [4, 4, 128])
    em.op('pool', lambda e: e.iota(iot[:, 0:128], [[1, 128]], base=0, channel_multiplier=-1), (), ['iot'])
    em.ts('dve', ident_f[:], iot[:, 0:128], 0.0, ALU.is_equal, ['iot'], ['ident_f'])
    em.cp('dve', ident_b[:], ident_f[:], ['ident_f'], ['ident_b'])
    em.ts('dve', tstrict[:], iot[:, 0:128], 0.0, ALU.is_gt, ['iot'], ['tstrict'])
    em.ts('dve', mmask[:], iot[:, 0:128], 0.0, ALU.is_ge, ['iot'], ['mmask'], s2=QSCALE, op1=ALU.mult)
    em.memset('dve', ones_b[:], 1.0, ['ones_b'])
    ones_f = sb("ones_f", [128, 128])
    em.memset('dve', ones_f[:], 1.0, ['ones_f'])
    for h_ in range(4):
        em.cp('dve', sel[:, h_, :], ident_f[0:4, h_:h_ + 1].broadcast_to([4, 128]), ['ident_f'], ['sel'])

    def adaln_phase():
        with ExitStack() as ar:
            def sa(name, shape, dt=F32):
                return ar.enter_context(nc.sbuf_tensor(name + '_u%d' % next(uid), list(shape), dt))
            condT = sa("condT", [128, 8])
            adw = [sa("adw%d" % i, [128, 8, 512]) for i in range(2)]
            modrow = sa("modrow", [1, 6 * D]); adb = sa("adb", [1, 6 * D])
            em.load('sp', condT[:], c_in.rearrange("o (kc p) -> p (o kc)", p=128), [], ['condT'])
            em.act(condT[:], condT[:], AF.Silu, ['condT'], ['condT'])
            for l in sorted(set(layers)):
                em.load('sp', adb[:], ada_b[l:l + 1, :], [], ['adb'])
                for n in range(12):
                    bf = adw[n % 2]
                    bn = 'adw%d' % (n % 2)
                    em.load('sp', bf[:], ada_w[l, :, n * 512:(n + 1) * 512].rearrange("(kc p) n -> p kc n", p=128), [], [bn])
                    for kc in range(8):
                        em.mm(pb[0][0:1, :], condT[:, kc:kc + 1], bf[:, kc, :], kc == 0, kc == 7, ['condT', bn], ['pb0'])
                    em.tt('dve', modrow[:, n * 512:(n + 1) * 512], pb[0][0:1, :], adb[:, n * 512:(n + 1) * 512], ALU.add, ['pb0', 'adb'], ['modrow'])
                em.load('sp', modv[l:l + 1, :], modrow[:], ['modrow'], ['modv'])
            em.fence()

    sc1p = sb("sc1p", [128, D]); sh1 = sb("sh1", [128, D]); g1b = sb("g1b", [128, D])
    sc2p = sb("sc2p", [128, D]); sh2 = sb("sh2", [128, D]); g2b = sb("g2b", [128, D])
    l1g = sb("l1g", [128, D]); l1b = sb("l1b", [128, D]); l2g = sb("l2g", [128, D]); l2b = sb("l2b", [128, D])
    wr = sb("wr", [128, 8, 36]); brb = sb("brb", [128, 36])
    xt = [sb("xt0", [128, D])] * 2
    tmpA = sb("tmpA", [128, D]); tmpB = sb("tmpB", [128, D])
    hb = sb("hb", [128, D], BF16)
    hT = sb("hT", [128, 8, 128], BF16)
    ub = sb("ub", [128, D], BF16)
    uT = sb("uT", [128, 8, 128], BF16)
    r1 = sb("r1", [128, D]); x1t = sb("x1t", [128, D]); h2f = tmpB; h2b = hb
    h2T = tmpA[:].rearrange("p (a b) -> p a b", a=8)
    stats = sb("stats", [128, 2, 6]); mv = sb("mv", [128, 2]); rstd = sb("rstd", [128, 1])
    lg = sb("lg", [128, 36]); sm = sb("sm", [128, 16]); goh = sb("goh", [128, 4]); esel = sb("esel", [128, 8]); esel2 = sb("esel2", [128, 8])
    oh1 = sb("oh1", [128, 8]); oh2 = sb("oh2", [128, 8]); gex = sb("gex", [128, 4])
    Mb = sb("Mb", [128, NE], BF16)
    running = sb("running", [128, NE])
    OH1 = sb("OH1", [128, NE]); OH2 = sb("OH2", [128, NE]); rankt = sb("rankt", [128, NE]); junk32 = sb("junk32", [128, NE])
    iota32 = sb("iota32", [128, NE])
    er_all = sb("er_all", [128, NT, 4])
    w_all = sb("w_all", [128, NT, 2])
    pos_f = sb("pos_f", [128, 2, NT]); pos_i = sb("pos_i", [128, 2, NT], I32)
    cnt_i = sb("cnt_i", [128, NE], I32); pend = sb("pend", [128, NE]); pstart = sb("pstart", [128, NE]); padf = sb("padf", [128, NE])
    zeros32 = sb("zeros32", [128, NE]); ones32 = sb("ones32", [128, NE])
    NBC = (NB + 127) // 128
    bstart = sb("bstart", [128, NBC]); blke = sb("blke", [128, NBC]); cmp32 = sb("cmp32", [128, NE])
    blke_row = sb("blke_row", [128, NB])
    wix_all = sb("wix_all", [128, 2 * NB], I32)
    wskip = sb("wskip", [128, NB]); wbase = sb("wbase", [128, NB])
    kofs = sb("kofs", [128, 12])
    ga = tmpA; gb = tmpB
    em.op('pool', lambda e: e.iota(iot[:, 0:NE], [[1, NE]], base=0, channel_multiplier=0), ['sel'], ['iot'])
    em.cp('dve', iota32[:], iot[:, 0:NE], ['iot'], ['iota32'])

    em.memset('dve', zeros32[:], 0.0, ['zeros32'])
    em.memset('dve', ones32[:], 1.0, ['ones32'])

    def load_bcast(dst, name, src_row):
        em.load('sp', dst[:], src_row.partition_broadcast(128), ['modv'], [name])

    def ln_tile(src, src_name, dst, dst_name, gam, gname, bet, bname):
        for hlf in range(2):
            em.op('dve', lambda e, hlf=hlf: e.bn_stats(out=stats[:, hlf, :], in_=src[:, hlf * 512:(hlf + 1) * 512]), [src_name], ['stats'])
        em.op('dve', lambda e: e.bn_aggr(out=mv[:], in_=stats[:].rearrange("p a b -> p (a b)")), ['stats'], ['mv'])
        em.act(rstd[:], mv[:, 1:2], AF.Sqrt, ['mv'], ['rstd'], bias=EPS)
        em.op('dve', lambda e: e.reciprocal(out=rstd[:], in_=rstd[:]), ['rstd'], ['rstd'])
        em.ts('dve', dst[:], src[:], mv[:, 0:1], ALU.subtract, [src_name, 'mv', 'rstd'], [dst_name], s2=rstd[:, 0:1], op1=ALU.mult)
        em.tt('pool', dst[:], dst[:], gam[:], ALU.mult, [dst_name, gname], [dst_name])
        em.tt('pool', dst[:], dst[:], bet[:], ALU.add, [dst_name, bname], [dst_name])

    def load_layer_common(l):
        for (dst, name, order) in [(sh1, 'sh1', 0), (sc1p, 'sc1p', 1), (g1b, 'g1b', 2), (sh2, 'sh2', 3), (sc2p, 'sc2p', 4)]:
            load_bcast(dst, name, modv[l, order * D:(order + 1) * D])
        em.ts('dve', sc1p[:], sc1p[:], 1.0, ALU.add, ['sc1p'], ['sc1p'])
        em.ts('dve', sc2p[:], sc2p[:], 1.0, ALU.add, ['sc2p'], ['sc2p'])
        em.load('sp', l1g[:], ln1_g[l, :].partition_broadcast(128), [], ['l1g'])
        em.load('sp', l1b[:], ln1_b[l, :].partition_broadcast(128), [], ['l1b'])
        with nc.allow_non_contiguous_dma(reason="small router weights"):
            em.load('sp', wr[:, :, 0:4], e_w_group[l].rearrange("(kc p) n -> p kc n", p=128), [], ['wr'])
            em.load('sp', wr[:, :, 4:36], e_w_router[l].rearrange("(kc p) n -> p kc n", p=128), [], ['wr'])
        em.load('sp', brb[:, 0:4], e_b_group[l, :].partition_broadcast(128), [], ['brb'])
        em.load('sp', brb[:, 4:36], e_b_router[l, :].partition_broadcast(128), [], ['brb'])
        em.memset('dve', running[:], 0.0, ['running'])

    def load_layer_tail(l):
        load_bcast(g2b, 'g2b', modv[l, 5 * D:6 * D])
        em.load('sp', l2g[:], ln2_g[l, :].partition_broadcast(128), [], ['l2g'])
        em.load('sp', l2b[:], ln2_b[l, :].partition_broadcast(128), [], ['l2b'])

    def front(ti, src_d, first):
        xb_ = xt[0]; xn = 'xt0'
        if first:
            em.load('sp', xb_[:], src_d[ti * 128:(ti + 1) * 128, :], [], [xn])
        else:
            combine_tile(ti, xb_, xn)
        return xb_, xn

    def combine_tile(ti, dst, dname):
        em.load('sp', x1t[:], x1d[ti * 128:(ti + 1) * 128, :], ['x1d'], ['x1t'])
        em.dma('pool', lambda e: e.indirect_dma_start(out=ga[:], out_offset=None, in_=ysd[:, :],
                                                      in_offset=bass.IndirectOffsetOnAxis(ap=pos_i[:, 0, ti:ti + 1], axis=0)),
               ['ysd', 'pos_i'], ['tmpA'])
        em.dma('pool', lambda e: e.indirect_dma_start(out=gb[:], out_offset=None, in_=ysd[:, :],
                                                      in_offset=bass.IndirectOffsetOnAxis(ap=pos_i[:, 1, ti:ti + 1], axis=0)),
               ['ysd', 'pos_i'], ['tmpB'])
        em.ts('dve', ga[:], ga[:], w_all[:, ti, 0:1], ALU.mult, ['tmpA', 'w_all'], ['tmpA'])
        em.stt(ga[:], gb[:], w_all[:, ti, 1:2], ga[:], ALU.mult, ALU.add, ['tmpA', 'tmpB', 'w_all'], ['tmpA'])
        em.tt('pool', ga[:], ga[:], g2b[:], ALU.mult, ['tmpA', 'g2b'], ['tmpA'])
        em.stt(r1[:], x1t[:], ALPHA, ga[:], ALU.mult, ALU.add, ['x1t', 'tmpA'], ['r1'])
        ln_tile(r1, 'r1', dst, dname, l2g, 'l2g', l2b, 'l2b')

    def modulate_T(xb_, xn):
        em.tt('dve', tmpA[:], xb_[:], sc1p[:], ALU.mult, [xn, 'sc1p'], ['tmpA'])
        em.tt('pool', hb[:], tmpA[:], sh1[:], ALU.add, ['tmpA', 'sh1'], ['hb'])
        pT = pb[0][:].bitcast(BF16)
        for kc in range(8):
            em.tr(pT[:, kc * 128:(kc + 1) * 128], hb[:, kc * 128:(kc + 1) * 128], ident_b[:], ['hb', 'ident_b'], ['pb0'])
        em.cp('act', hT[:].rearrange("p a b -> p (a b)"), pT, ['pb0'], ['hT'])

    def post(ti, xb_, xn, Wo, won):
        pT = pb[0][:].bitcast(BF16)
        for kc in range(8):
            em.tr(pT[:, kc * 128:(kc + 1) * 128], ub[:, kc * 128:(kc + 1) * 128], ident_b[:], ['ub', 'ident_b'], ['pb0'])
        em.cp('act', uT[:].rearrange("p a b -> p (a b)"), pT, ['pb0'], ['uT'])
        for n in range(2):
            for kc in range(8):
                em.mm(pb[3 + n][:, :], uT[:, kc, :], Wo[:, kc, n * 512:(n + 1) * 512], kc == 0, kc == 7, ['uT', won], ['pb%d' % (3 + n)])
            em.tt('dve', tmpA[:, n * 512:(n + 1) * 512], pb[3 + n][:, :], g1b[:, n * 512:(n + 1) * 512], ALU.mult, ['pb%d' % (3 + n), 'g1b'], ['tmpA'])
        after_mix(ti, xb_, xn)

    def after_mix(ti, xb_, xn):
        em.stt(r1[:], xb_[:], ALPHA, tmpA[:], ALU.mult, ALU.add, [xn, 'tmpA'], ['r1'])
        ln_tile(r1, 'r1', x1t, 'x1t', l1g, 'l1g', l1b, 'l1b')
        em.load('sp', x1d[ti * 128:(ti + 1) * 128, :], x1t[:], ['x1t'], ['x1d'])
        em.tt('dve', tmpB[:], x1t[:], sc2p[:], ALU.mult, ['x1t', 'sc2p'], ['tmpB'])
        em.tt('pool', h2f[:], tmpB[:], sh2[:], ALU.add, ['tmpB', 'sh2'], ['tmpB'])
        em.cp('act', h2b[:], h2f[:], ['tmpB'], ['hb'])
        em.load('sp', h2d[ti * 128:(ti + 1) * 128, :], h2b[:], ['hb'], ['h2d'])
        for kc in range(8):
            bk = 1 + kc // 4
            em.tr(pb[bk][:, (kc % 4) * 128:(kc % 4 + 1) * 128], h2f[:, kc * 128:(kc + 1) * 128], ident_f[:], ['tmpB', 'ident_f'], ['pb%d' % bk])
        em.cp('act', tmpA[:, 0:512], pb[1][:, :], ['pb1'], ['tmpA'])
        em.cp('act', tmpA[:, 512:1024], pb[2][:, :], ['pb2'], ['tmpA'])
        for kc in range(8):
            em.mm(pb[7][:, 324:360], h2T[:, kc, :], wr[:, kc, :], kc == 0, kc == 7, ['tmpA', 'wr'], ['pb7'])
        em.tt('dve', lg[:], pb[7][:, 324:360], brb[:], ALU.add, ['pb7', 'brb'], ['lg'])
        em.red(sm[:, 0:1], lg[:, 0:4], ALU.max, ['lg'], ['sm'])
        em.ts('dve', goh[:], lg[:, 0:4], sm[:, 0:1], ALU.is_equal, ['lg', 'sm'], ['goh'])
        em.ts('dve', sm[:, 1:2], sm[:, 0:1], -1.0, ALU.mult, ['sm'], ['sm'])
        em.act(gex[:], lg[:, 0:4], AF.Exp, ['lg', 'sm'], ['gex', 'sm'], bias=sm[:, 1:2], accum=sm[:, 2:3])
        em.op('dve', lambda e: e.reciprocal(out=sm[:, 3:4], in_=sm[:, 2:3]), ['sm'], ['sm'])
        em.ts('dve', esel[:], lg[:, 4:12], goh[:, 0:1], ALU.mult, ['lg', 'goh'], ['esel'])
        for g in range(1, 4):
            em.stt(esel[:], lg[:, 4 + 8 * g:12 + 8 * g], goh[:, g:g + 1], esel[:], ALU.mult, ALU.add, ['lg', 'goh', 'esel'], ['esel'])
        em.red(sm[:, 4:5], esel[:], ALU.max, ['esel'], ['sm'])
        em.ts('dve', oh1[:], esel[:], sm[:, 4:5], ALU.is_equal, ['esel', 'sm'], ['oh1'])
        em.stt(esel2[:], oh1[:], -1e30, esel[:], ALU.mult, ALU.add, ['oh1', 'esel'], ['esel2'])
        em.red(sm[:, 5:6], esel2[:], ALU.max, ['esel2'], ['sm'])
        em.ts('dve', oh2[:], esel2[:], sm[:, 5:6], ALU.is_equal, ['esel2', 'sm'], ['oh2'])
        em.tt('dve', sm[:, 6:7], sm[:, 5:6], sm[:, 4:5], ALU.subtract, ['sm'], ['sm'])
        em.act(sm[:, 7:8], sm[:, 6:7], AF.Exp, ['sm'], ['sm'])
        em.ts('dve', sm[:, 8:9], sm[:, 7:8], 1.0, ALU.add, ['sm'], ['sm'])
        em.op('dve', lambda e: e.reciprocal(out=sm[:, 9:10], in_=sm[:, 8:9]), ['sm'], ['sm'])
        em.tt('dve', w_all[:, ti, 0:1], sm[:, 3:4], sm[:, 9:10], ALU.mult, ['sm'], ['w_all'])
        em.tt('dve', w_all[:, ti, 1:2], sm[:, 3:4], w_all[:, ti, 0:1], ALU.subtract, ['sm', 'w_all'], ['w_all'])
        gbc = goh[:].unsqueeze(2).broadcast_to([128, 4, 8])
        em.tt('dve', OH1[:].rearrange("p (g e) -> p g e", g=4), gbc, oh1[:].unsqueeze(1).broadcast_to([128, 4, 8]), ALU.mult, ['goh', 'oh1'], ['OH1'])
        em.tt('dve', OH2[:].rearrange("p (g e) -> p g e", g=4), gbc, oh2[:].unsqueeze(1).broadcast_to([128, 4, 8]), ALU.mult, ['goh', 'oh2'], ['OH2'])
        em.tt('dve', Mb[:], OH1[:], OH2[:], ALU.add, ['OH1', 'OH2'], ['Mb'])
        em.mm(pb[7][:, 260:292], tstrict[:], Mb[:], True, True, ['tstrict', 'Mb'], ['pb7'])
        em.tt('dve', rankt[:], pb[7][:, 260:292], running[:], ALU.add, ['pb7', 'running'], ['rankt'])
        em.mm(pb[7][:, 292:324], ones_b[:], Mb[:], True, True, ['ones_b', 'Mb'], ['pb7'])
        em.tt('dve', running[:], pb[7][:, 292:324], running[:], ALU.add, ['pb7', 'running'], ['running'])
        for k_, OHk in enumerate((OH1, OH2)):
            nm = 'OH%d' % (k_ + 1)
            em.tt('dve', junk32[:], OHk[:], iota32[:], ALU.mult, [nm, 'iota32'], ['junk32'])
            em.red(er_all[:, ti, k_:k_ + 1], junk32[:], ALU.add, ['junk32'], ['er_all'])
            em.tt('dve', junk32[:], OHk[:], rankt[:], ALU.mult, [nm, 'rankt'], ['junk32'])
            em.red(er_all[:, ti, 2 + k_:3 + k_], junk32[:], ALU.add, ['junk32'], ['er_all'])

    def moe(l):
        with ExitStack() as ar:
            def sa(name, shape, dt=F32):
                return ar.enter_context(nc.sbuf_tensor(name + '_u%d' % next(uid), list(shape), dt))
            xsb = [sa("xsb%d" % i, [128, D], BF16) for i in range(2)]
            xsT = [sa("xsT%d" % i, [128, 8, 128], BF16) for i in range(2)]
            w1b = [sa("w1b%d" % i, [128, 8, DE], BF16) for i in range(2)]
            w3b = [sa("w3b%d" % i, [128, 8, DE], BF16) for i in range(2)]
            w2b = [sa("w2b%d" % i, [128, 4, D], BF16) for i in range(2)]
            s1 = [sa("s1_%d" % i, [128, DE]) for i in range(2)]; ab = [sa("ab%d" % i, [128, DE], BF16) for i in range(2)]
            aT = [sa("aT%d" % i, [128, 4, 128], BF16) for i in range(2)]
            ysb = [sa("ysb%d" % i, [128, D]) for i in range(2)]
            moe_body(l, xsb, xsT, w1b, w3b, w2b, s1, ab, aT, ysb)
            em.fence()

    def moe_body(l, xsb, xsT, w1b, w3b, w2b, s1, ab, aT, ysb):
        em.ts('dve', padf[:], running[:], float(BK - 1), ALU.add, ['running'], ['padf'])
        em.cp('dve', cnt_i[:], padf[:], ['padf'], ['cnt_i'])
        em.op('dve', lambda e: e.tensor_scalar(out=cnt_i[:], in0=cnt_i[:], scalar1=8, scalar2=None, op0=ALU.arith_shift_right), ['cnt_i'], ['cnt_i'])
        em.op('dve', lambda e: e.tensor_scalar(out=cnt_i[:], in0=cnt_i[:], scalar1=8, scalar2=None, op0=ALU.logical_shift_left), ['cnt_i'], ['cnt_i'])
        em.cp('dve', padf[:], cnt_i[:], ['cnt_i'], ['padf'])
        em.op('dve', lambda e: e.tensor_tensor_scan(out=pend[:], data0=ones32[:], data1=padf[:], initial=0.0, op0=ALU.mult, op1=ALU.add), ['ones32', 'padf'], ['pend'])
        em.tt('dve', pstart[:], pend[:], padf[:], ALU.subtract, ['pend', 'padf'], ['pstart'])
        for ti in range(NT):
            for k_ in range(2):
                em.stt(junk32[:], iota32[:], er_all[:, ti, k_:k_ + 1], pstart[:], ALU.is_equal, ALU.mult, ['iota32', 'er_all', 'pstart'], ['junk32'])
                em.red(pos_f[:, k_, ti:ti + 1], junk32[:], ALU.add, ['junk32'], ['pos_f'])
        em.tt('dve', pos_f[:], pos_f[:], er_all[:, :, 2:4].rearrange("p t k -> p k t"), ALU.add, ['pos_f', 'er_all'], ['pos_f'])
        em.cp('dve', pos_i[:], pos_f[:], ['pos_f'], ['pos_i'])
        em.op('pool', lambda e: e.iota(iot[:, 0:NBC], [[128, NBC]], base=0, channel_multiplier=1), ['sel'], ['iot'])
        em.cp('dve', bstart[:], iot[:, 0:NBC], ['iot'], ['bstart'])
        em.ts('dve', bstart[:], bstart[:], float(BK), ALU.mult, ['bstart'], ['bstart'])
        for cix in range(NBC):
            em.ts('dve', cmp32[:], pend[:], bstart[:, cix:cix + 1], ALU.is_le, ['pend', 'bstart'], ['cmp32'])
            em.red(blke[:, cix:cix + 1], cmp32[:], ALU.add, ['cmp32'], ['blke'])
        em.ts('dve', blke[:], blke[:], float(NE - 1), ALU.min, ['blke'], ['blke'])
        for cix in range(NBC):
            nb_c = min(128, NB - cix * 128)
            em.ts('dve', tmpB[:, 0:128], ident_f[:], blke[:, cix:cix + 1], ALU.mult, ['ident_f', 'blke'], ['tmpB'])
            em.mm(pb[7][:, 0:128], ones_f[:], tmpB[:, 0:128], True, True, ['ones_f', 'tmpB'], ['pb7'])
            em.cp('dve', blke_row[:, cix * 128:cix * 128 + nb_c], pb[7][:, 0:nb_c], ['pb7'], ['blke_row'])
        em.op('pool', lambda e: e.iota(iot[:, 0:1], [[0, 1]], base=l * NE * 256, channel_multiplier=2), ['bstart'], ['iot'])
        em.cp('dve', kofs[:, 0:1], iot[:, 0:1], ['iot'], ['kofs'])
        em.memset('dve', wskip[:], 0.0, ['wskip'])
        em.tt('dve', wskip[:, 2:NB], blke_row[:, 2:NB], blke_row[:, 0:NB - 2], ALU.is_equal, ['blke_row', 'wskip'], ['wskip'])
        em.ts('dve', wbase[:], blke_row[:], 256.0, ALU.mult, ['blke_row'], ['wbase'], s2=kofs[:, 0:1], op1=ALU.add)
        em.stt(wbase[:], wskip[:], 1.0e6, wbase[:], ALU.mult, ALU.add, ['wskip', 'wbase'], ['wbase'])
        em.cp('dve', wix_all[:, 0:NB], wbase[:], ['wbase'], ['wix_all'])
        em.ts('dve', wbase[:], wbase[:], 1.0, ALU.add, ['wbase'], ['wbase'])
        em.cp('dve', wix_all[:, NB:2 * NB], wbase[:], ['wbase'], ['wix_all'])
        for ti in range(NT):
            bf = xsb[ti % 2]; bn = 'xsb%d' % (ti % 2)
            em.load('sp', bf[:], h2d[ti * 128:(ti + 1) * 128, :], ['h2d'], [bn])
            for k in range(2):
                em.dma('pool', lambda e, ti=ti, k=k, bf=bf: e.indirect_dma_start(
                    out=xsd[:, :], out_offset=bass.IndirectOffsetOnAxis(ap=pos_i[:, k, ti:ti + 1], axis=0),
                    in_=bf[:], in_offset=None), [bn, 'pos_i'], ['xsd'])
        w1v = e_w1; w3v = e_w3; w2v = e_w2

        WMAX = DEPTH * NE * 256 - 1

        def getreg(e, val):
            if val not in regcache:
                regcache[val] = e.to_reg(val)
            return regcache[val]

        def issue_w(b):
            i = b % 2
            for (wv_, wb_, wname) in ((w1v, w1b, 'w1b'), (w3v, w3b, 'w3b'), (w2v, w2b, 'w2b')):
                for hf in range(2):
                    dst = wb_[i][:].rearrange("p a b -> p (a b)")[:, hf * 2048:(hf + 1) * 2048]
                    em.dma('pool', lambda e, b=b, hf=hf, dst=dst, wv_=wv_: e.indirect_dma_start(
                        out=dst, out_offset=None, in_=wv_[:, :],
                        in_offset=bass.IndirectOffsetOnAxis(ap=wix_all[:, hf * NB + b:hf * NB + b + 1], axis=0),
                        bounds_check=getreg(e, WMAX), oob_is_err=False), ['wix_all'], ['%s%d' % (wname, i)])

        issue_w(0)
        for b in range(NB if not KNOBS.get('no_blocks') else 0):
            i = b % 2
            if b + 1 < NB:
                issue_w(b + 1)
            for sub in range(SUB):
                jb = (b * SUB + sub)
                jp = jb % 2
                bf = xsb[jp]; bn = 'xsb%d' % jp
                xT_ = xsT[jp]; xTn = 'xsT%d' % jp
                tb0 = 0 if jp == 0 else 6
                tb1 = 5 if jp == 0 else 7
                pT = pb[tb0][:].bitcast(BF16)
                em.load('sp', bf[:], xsd[jb * 128:(jb + 1) * 128, :], ['xsd'], [bn])
                for kc in range(8):
                    em.tr(pT[:, kc * 128:(kc + 1) * 128], bf[:].rearrange("t (p k) -> t k p", k=8)[:, kc, :], ident_b[:], [bn, 'ident_b'], ['pb%d' % tb0])
                em.cp('act', xT_[:].rearrange("p a b -> p (a b)"), pT, ['pb%d' % tb0], [xTn])
                for kc in range(8):
                    em.mm(pb[1][:, :], xT_[:, kc, :], w1b[i][:, kc, :], kc == 0, kc == 7, [xTn, 'w1b%d' % i], ['pb1'])
                for kc in range(8):
                    em.mm(pb[2][:, :], xT_[:, kc, :], w3b[i][:, kc, :], kc == 0, kc == 7, [xTn, 'w3b%d' % i], ['pb2'])
                em.act(s1[jp][:], pb[1][:, :], AF.Silu, ['pb1'], ['s1_%d' % jp])
                em.tt('dve', ab[jp][:], s1[jp][:], pb[2][:, :], ALU.mult, ['s1_%d' % jp, 'pb2'], ['ab%d' % jp])
                pT2 = pb[tb1][:].bitcast(BF16)
                for fc in range(4):
                    em.tr(pT2[:, fc * 128:(fc + 1) * 128], ab[jp][:].rearrange("t (p k) -> t k p", k=4)[:, fc, :], ident_b[:], ['ab%d' % jp, 'ident_b'], ['pb%d' % tb1])
                em.cp('act', aT[jp][:].rearrange("p a b -> p (a b)"), pT2[:, 0:512], ['pb%d' % tb1], ['aT%d' % jp])
                for n in range(2):
                    for fc in range(4):
                        em.mm(pb[3 + n][:, :], aT[jp][:, fc, :], w2b[i][:, fc, n * 512:(n + 1) * 512], fc == 0, fc == 3, ['aT%d' % jp, 'w2b%d' % i], ['pb%d' % (3 + n)])
                yb = ysb[jp]; yn = 'ysb%d' % jp
                em.cp('dve', yb[:, 0:512], pb[3][:, :], ['pb3'], [yn])
                em.cp('act', yb[:, 512:1024], pb[4][:, :], ['pb4'], [yn])
                em.load('sp', ysd[jb * 128:(jb + 1) * 128, :], yb[:], [yn], ['ysd'])

    def mlstm_layer(l, first):
        with ExitStack() as ar:
            def sa(name, shape, dt=F32):
                return ar.enter_context(nc.sbuf_tensor(name + '_u%d' % next(uid), list(shape), dt))
            Wqk = sa("Wqk", [128, 8, 1024], BF16); Wvo = sa("Wvo", [128, 8, 2048], BF16); Wg = sa("Wg", [128, 8, 8], BF16)
            Wout = sa("Wout", [128, 8, 1024], BF16)
            cw = sa("cw", [128, 8, 4]); cbias = sa("cbias", [128, 8]); bif = sa("bif", [4, 2]); nbf = sa("nbf", [4, 1])
            normg = sa("normg", [128, D])
            qkraw = sa("qkraw", [128, 8, 131]); cacc = sa("cacc", [128, 8, 128]); ctmp = tmpA[:].rearrange("p (a b) -> p a b", a=8)
            qkT = sa("qkT", [128, 8, 128], BF16)
            vext = sa("vext", [128, 4, 257], BF16)
            sigo = sa("sigo", [128, D])
            grow = [sa("grow%d" % i, [4, 6, 128]) for i in range(2)]
            ones4 = sa("ones4", [4, 128])
            tm12 = sa("tm12", [128, 12])
            gends = sa("gends", [128, 4, 2])
            nge = sa("nge", [128, 4])
            DT = sa("DT", [128, 4, 128])
            wT_ = [sa("wT%d" % i, [128, 128], BF16) for i in range(2)]
            Asb_ = [sa("Asb%d" % i, [128, 257]) for i in range(2)]; num_ = [sa("num%d" % i, [128, 257]) for i in range(2)]
            eint = sa("eint", [128, 4]); flo = sa("flo", [128, 4]); dmax_ = [sa("dmax%d" % i, [128, 1]) for i in range(2)]; rr_m = [sa("rr%d" % i, [128, 1]) for i in range(2)]
            hh = tmpB
            st2_ = [sa("st2_%d" % i, [128, 6]) for i in range(2)]; mv2_ = [sa("mv2_%d" % i, [128, 2]) for i in range(2)]; rs2_ = [sa("rs2_%d" % i, [128, 1]) for i in range(2)]
            scl = sa("scl", [128, 4]); dec = sa("dec", [128, 4])
            wk_ = [sa("wk%d" % i, [128, 128], BF16) for i in range(2)]
            Cst = [sa("Cst%d" % h, [128, 257]) for h in range(4)]
            Cbf = [sa("Cbf%d" % h, [128, 257], BF16) for h in range(4)]
            def mlstm_setup(j):
                wv = m_w_in[j].rearrange("(kc p) n -> p kc n", p=128)
                em.load('pool', Wqk[:], wv[:, :, 0:1024], [], ['Wqk'])
                em.load('pool', Wvo[:, :, 0:1024], wv[:, :, 1024:2048], [], ['Wvo'])
                em.load('pool', Wvo[:, :, 1024:2048], wv[:, :, 2048:3072], [], ['Wvo'])
                with nc.allow_non_contiguous_dma(reason="small gate weights"):
                    em.load('pool', Wg[:], wv[:, :, 3072:3080], [], ['Wg'])
                    for jj_ in range(4):
                        em.load('sp', cw[:, :, jj_], m_conv_w[j, jj_].rearrange("(cc p) -> p cc", p=128), [], ['cw'])
                    em.load('sp', cbias[:], m_conv_b[j].rearrange("(cc p) -> p cc", p=128), [], ['cbias'])
                    em.load('sp', bif[:], m_b_if[j].rearrange("(a h) -> h a", a=2), [], ['bif'])
                em.load('pool', Wout[:], m_w_out[j].rearrange("(kc p) n -> p kc n", p=128), [], ['Wout'])
                em.load('sp', normg[:], m_norm_g[j, :].partition_broadcast(128), [], ['normg'])
                em.ts('dve', nbf[:], bif[:, 1:2], -1.0, ALU.mult, ['bif'], ['nbf'])
                em.memset('dve', qkraw[:], 0.0, ['qkraw'])
                em.memset('dve', vext[:], 1.0, ['vext'])
                em.memset('dve', ones4[:], 1.0, ['ones4'])
                em.memset('dve', gends[:], 0.0, ['gends'])
                em.memset('dve', grow[1][:], 0.0, ['grow1'])
                for h in range(4):
                    em.memset('dve', Cst[h][:], 0.0, ['Cst%d' % h])
                    em.memset('pool', Cbf[h][:], 0.0, ['Cbf%d' % h])

            def mlstm_tile(ti):
                gr = grow[ti % 2]; gn = 'grow%d' % (ti % 2); gp = grow[(ti + 1) % 2]; gpn = 'grow%d' % ((ti + 1) % 2)
                for cc in range(8):
                    bk = 1 + cc // 4
                    for kc in range(8):
                        em.mm(pb[bk][:, (cc % 4) * 128:(cc % 4 + 1) * 128], Wqk[:, kc, cc * 128:(cc + 1) * 128], hT[:, kc, :], kc == 0, kc == 7, ['Wqk', 'hT'], ['pb%d' % bk])
                em.cp('act', qkraw[:, 0:4, 3:131], pb[1][:, :].rearrange("p (a b) -> p a b", a=4), ['pb1'], ['qkraw'])
                em.cp('act', qkraw[:, 4:8, 3:131], pb[2][:, :].rearrange("p (a b) -> p a b", a=4), ['pb2'], ['qkraw'])
                def wbc(jj):
                    return cw[:, :, jj:jj + 1].broadcast_to([128, 8, 128])
                em.tt('dve', cacc[:], qkraw[:, :, 3:131], wbc(3), ALU.mult, ['qkraw', 'cw'], ['cacc'])
                for jj in (2, 1, 0):
                    em.tt('pool', ctmp[:], qkraw[:, :, jj:jj + 128], wbc(jj), ALU.mult, ['qkraw', 'cw'], ['ctmp'])
                    em.tt('dve', cacc[:], cacc[:], ctmp[:], ALU.add, ['cacc', 'ctmp'], ['cacc'])
                em.tt('dve', cacc[:], cacc[:], cbias[:].unsqueeze(2).broadcast_to([128, 8, 128]), ALU.add, ['cacc', 'cbias'], ['cacc'])
                em.cp('pool', qkraw[:, :, 0:3], qkraw[:, :, 128:131], ['qkraw'], ['qkraw'])
                em.act(qkT[:], cacc[:], AF.Silu, ['cacc'], ['qkT'])
                for n4 in range(4):
                    bk = 3 + n4 % 2
                    for kc in range(8):
                        em.mm(pb[bk][:, :], hT[:, kc, :], Wvo[:, kc, n4 * 512:(n4 + 1) * 512], kc == 0, kc == 7, ['hT', 'Wvo'], ['pb%d' % bk])
                    if n4 < 2:
                        em.cp('act', vext[:, 2 * n4:2 * n4 + 2, 0:256], pb[bk][:, :].rearrange("p (a b) -> p a b", a=2), ['pb%d' % bk], ['vext'])
                    else:
                        em.act(sigo[:, (n4 - 2) * 512:(n4 - 1) * 512], pb[bk][:, :], AF.Sigmoid, ['pb%d' % bk], ['sigo'])
                for kc in range(8):
                    em.mm(pb[6][0:4, 0:128], Wg[:, kc, 0:4], hT[:, kc, :], kc == 0, kc == 7, ['Wg', 'hT'], ['pb6'])
                for kc in range(8):
                    em.mm(pb[6][0:4, 128:256], Wg[:, kc, 4:8], hT[:, kc, :], kc == 0, kc == 7, ['Wg', 'hT'], ['pb6'])
                em.act(gr[:, 0, :], pb[6][0:4, 128:256], AF.Exp, ['pb6', 'nbf'], [gn], bias=nbf[:, 0:1], scale=-1.0)
                em.act(gr[:, 0, :], gr[:, 0, :], AF.Ln, [gn], [gn], bias=1.0)
                em.act(gr[:, 2, :], pb[6][0:4, 0:128], AF.Identity, ['pb6', 'bif'], [gn], bias=bif[:, 0:1])
                em.op('dve', lambda e: e.tensor_tensor_scan(out=gr[:, 1, :], data0=ones4[:], data1=gr[:, 0, :], initial=gp[:, 1, 127:128],
                                                            op0=ALU.mult, op1=ALU.add), [gn, gpn, 'ones4'], [gn])
                em.tt('dve', gr[:, 3, :], gr[:, 2, :], gr[:, 1, :], ALU.add, [gn], [gn])
                em.op('dve', lambda e: e.tensor_tensor_scan(out=gr[:, 4, :], data0=gr[:, 3, :], data1=gr[:, 3, :], initial=gp[:, 4, 127:128],
                                                            op0=ALU.max, op1=ALU.max), [gn, gpn], [gn])
                em.tt('dve', gr[:, 5, :], gr[:, 1, :], gr[:, 4, :], ALU.subtract, [gn], [gn])
                for q_, row in enumerate((3, 4, 5)):
                    em.tr(pb[6][:, 256 + 4 * q_:260 + 4 * q_], gr[:, row, :], ident_f[0:4, 0:4], [gn, 'ident_f'], ['pb6'])
                em.cp('dve', tm12[:], pb[6][:, 256:268], ['pb6'], ['tm12'])
                for h in range(4):
                    em.mm(pb[5][:, h * 128:(h + 1) * 128], sel[:, h, :], gr[:, 4, :], True, True, ['sel', gn], ['pb5'])
                em.cp('pool', gends[:, :, 0:1], gends[:, :, 1:2], ['gends'], ['gends'])
                em.cp('dve', gends[:, :, 1:2], pb[5][:, :].rearrange("p (h t) -> p h t", h=4)[:, :, 127:128], ['pb5'], ['gends'])
                em.ts('dve', nge[:], gends[:, :, 1], -1.0, ALU.mult, ['gends'], ['nge'], s2=math.log(QSCALE), op1=ALU.add)
                for h in range(4):
                    em.act(DT[:, h, :], pb[5][:, h * 128:(h + 1) * 128], AF.Exp, ['pb5', 'tm12'], ['DT'], bias=tm12[:, h:h + 1], scale=-1.0)
                em.tt('pool', DT[:], DT[:], mmask[:].unsqueeze(1).broadcast_to([128, 4, 128]), ALU.mult, ['DT', 'mmask'], ['DT'])
                for h in range(4):
                    em.act(eint[:, h:h + 1], tm12[:, 4 + h:5 + h], AF.Exp, ['tm12', 'gends'], ['eint'], bias=gends[:, h, 0:1], scale=-1.0)
                    em.act(flo[:, h:h + 1], tm12[:, 8 + h:9 + h], AF.Exp, ['tm12'], ['flo'])
                    em.act(scl[:, h:h + 1], tm12[:, h:h + 1], AF.Exp, ['tm12', 'nge'], ['scl'], bias=nge[:, h:h + 1])
                    em.act(dec[:, h:h + 1], gends[:, h, 1:2], AF.Exp, ['gends'], ['dec'], bias=gends[:, h, 0:1], scale=-1.0)
                em.memset('dve', rr_m[0][:], 0.0, ['tmpB', 'hh0', 'hh1', 'hh2', 'hh3', 'rr0'])
                def head_gen(h):
                    cs = 'Cst%d' % h; cb_ = 'Cbf%d' % h
                    hp = h % 2; sx = str(hp)
                    wT = wT_[hp]; Asb = Asb_[hp]; num = num_[hp]; dmax = dmax_[hp]; rr = rr_m[hp]; st2 = st2_[hp]; mv2 = mv2_[hp]; rs2 = rs2_[hp]; wk = wk_[hp]
                    bS, bA, bB, bC = (6, 7, 1, 2) if hp == 0 else (0, 5, 3, 4)
                    nS, nA, nB, nC = 'pb%d' % bS, 'pb%d' % bA, 'pb%d' % bB, 'pb%d' % bC
                    em.mm(pb[bS][:, 384:512], qkT[:, 4 + h, :], qkT[:, h, :], True, True, ['qkT'], [nS])
                    yield
                    em.tt('dve', wT[:], pb[bS][:, 384:512], DT[:, h, :], ALU.mult, [nS, 'DT'], ['wT' + sx])
                    yield
                    em.mm(pb[bA][:, 0:257], wT[:], vext[:, h, :], True, True, ['wT' + sx, 'vext'], [nA])
                    em.mm(pb[bB][:, 0:257], qkT[:, h, :], Cbf[h][:], True, True, ['qkT', cb_], [nB])
                    yield
                    em.cp('act', Asb[:], pb[bA][:, 0:257], [nA], ['Asb' + sx])
                    yield
                    em.stt(num[:], pb[bB][:, 0:257], eint[:, h:h + 1], Asb[:], ALU.mult, ALU.add, [nB, 'eint', 'Asb' + sx], ['num' + sx])
                    em.ts('dve', dmax[:], num[:, 256:257], -1.0, ALU.mult, ['num' + sx], ['dmax' + sx])
                    em.tt('dve', dmax[:], dmax[:], num[:, 256:257], ALU.max, ['dmax' + sx, 'num' + sx], ['dmax' + sx])
                    em.tt('dve', dmax[:], dmax[:], flo[:, h:h + 1], ALU.max, ['dmax' + sx, 'flo'], ['dmax' + sx])
                    yield
                    em.op('dve', lambda e, rr=rr, dmax=dmax: e.reciprocal(out=rr[:], in_=dmax[:]), ['dmax' + sx], ['rr' + sx])
                    hn_ = 'hh%d' % h
                    em.ts('dve', hh[:, h * 256:(h + 1) * 256], num[:, 0:256], rr[:, 0:1], ALU.mult, ['num' + sx, 'rr' + sx], [hn_])
                    yield
                    em.op('dve', lambda e, h=h, st2=st2: e.bn_stats(out=st2[:], in_=hh[:, h * 256:(h + 1) * 256]), [hn_], ['st2' + sx])
                    em.op('dve', lambda e, st2=st2, mv2=mv2: e.bn_aggr(out=mv2[:], in_=st2[:]), ['st2' + sx], ['mv2' + sx])
                    yield
                    em.act(rs2[:], mv2[:, 1:2], AF.Sqrt, ['mv2' + sx], ['rs2' + sx], bias=EPS)
                    em.op('dve', lambda e, rs2=rs2: e.reciprocal(out=rs2[:], in_=rs2[:]), ['rs2' + sx], ['rs2' + sx])
                    em.ts('dve', hh[:, h * 256:(h + 1) * 256], hh[:, h * 256:(h + 1) * 256], mv2[:, 0:1], ALU.subtract, [hn_, 'mv2' + sx, 'rs2' + sx], [hn_], s2=rs2[:, 0:1], op1=ALU.mult)
                    yield
                    pK = pb[bS][:].bitcast(BF16)[:, 640:768]
                    em.tr(pK, qkT[:, 4 + h, :], ident_b[:], ['qkT', 'ident_b'], [nS])
                    yield
                    em.act(wk[:], pK, AF.Copy, [nS, 'scl'], ['wk' + sx], scale=scl[:, h:h + 1])
                    yield
                    em.mm(pb[bC][:, 0:257], wk[:], vext[:, h, :], True, True, ['wk' + sx, 'vext'], [nC])
                    yield
                    em.stt(Cst[h][:], Cst[h][:], dec[:, h:h + 1], pb[bC][:, 0:257], ALU.mult, ALU.add, [cs, 'dec', nC], [cs])
                    em.cp('pool', Cbf[h][:], Cst[h][:], [cs], [cb_])

                    yield
                for pair in ((0, 1), (2, 3)):
                    gens = [head_gen(h_) for h_ in pair]
                    alive = True
                    while alive:
                        alive = False
                        for g_ in gens:
                            try:
                                next(g_)
                                alive = True
                            except StopIteration:
                                pass
                em.tt('pool', hh[:], hh[:], normg[:], ALU.mult, ['hh0', 'hh1', 'hh2', 'hh3', 'tmpB', 'normg'], ['tmpB', 'hh0', 'hh1', 'hh2', 'hh3'])
                em.tt('dve', ub[:], hh[:], sigo[:], ALU.mult, ['tmpB', 'sigo'], ['ub'])


            load_layer_common(l)
            mlstm_setup(l // 3)
            for ti in range(NT):
                xb_, xn = front(ti, x_in, first)
                modulate_T(xb_, xn)
                mlstm_tile(ti)
                post(ti, xb_, xn, Wout, 'Wout')
            load_layer_tail(l)
            em.fence()

    def s5_layer(l, first):
        j = l // 3
        N = 128
        TWO_PI = 2.0 * math.pi
        C1 = 6.28125
        C2 = TWO_PI - C1
        with ExitStack() as ar:
            def sa(name, shape, dt=F32):
                return ar.enter_context(nc.sbuf_tensor(name + '_u%d' % next(uid), list(shape), dt))
            cosT = sa("cosT", [128, 32, N]); sinT = sa("sinT", [128, 32, N])
            magT = sa("magT", [128, 32])
            BBre = sa("BBre", [128, 8, 2, 128], BF16); BBim = sa("BBim", [128, 8, 2, 128], BF16)
            CCre = sa("CCre", [128, 32, 32]); CCimN = sa("CCimN", [128, 32, 32])
            dsb = sa("dsb", [128, D])
            xprev = sa("xprev", [128, 2, 32])
            with ExitStack() as ar2:
                def st_(name, shape, dt=F32):
                    return ar2.enter_context(nc.sbuf_tensor(name + '_u%d' % next(uid), list(shape), dt))
                lrT = st_("lrT", [128, 32]); limT = st_("limT", [128, 32]); ldtT = st_("ldtT", [128, 32]); thT = st_("thT", [128, 32])
                jrow = st_("jrow", [128, N])
                phi = st_("phi", [128, 16, N]); kk = st_("kk", [128, 16, N]); rr_ = st_("rr_", [128, 16, N])
                ab_re = st_("ab_re", [128, 32]); ab_im = st_("ab_im", [128, 32]); tre = st_("tre", [128, 32]); tim = st_("tim", [128, 32])
                q1 = st_("q1", [128, 32]); q2 = st_("q2", [128, 32])
                brT = st_("brT", [128, 32, 16]); biT = st_("biT", [128, 32, 16]); bbr = st_("bbr", [128, 32, 16]); bbi = st_("bbi", [128, 32, 16]); bt = st_("bt", [128, 32, 16])
                Xre = st_("Xre", [128, 8, 128], BF16); Xim = st_("Xim", [128, 8, 128], BF16)
                Cn_re = st_("Cn_re", [128, 8, 64]); Cn_im = st_("Cn_im", [128, 8, 64]); Yr = st_("Yr", [128, 8, 128]); Yi = st_("Yi", [128, 8, 128])
                mki = st_("mki", [128, 1], I32); m1 = st_("m1", [128, 1]); m0 = st_("m0", [128, 1])
                em.load('sp', lrT[:], s_lam_re[j].rearrange("(st two) p -> (two p) st", two=2), [], ['lrT'])
                em.load('sp', limT[:], s_lam_im[j].rearrange("(st two) p -> (two p) st", two=2), [], ['limT'])
                ldv = s_log_dt[j].rearrange("(st two) -> two st", two=2)
                for half in range(2):
                    em.load('sp', ldtT[half * 64:(half + 1) * 64, :], ldv[half].partition_broadcast(64), [], ['ldtT'])
                em.load('sp', brT[:], s_b_re[j].rearrange("(st two) p i -> (two p) st i", two=2), [], ['brT'])
                em.load('sp', biT[:], s_b_im[j].rearrange("(st two) p i -> (two p) st i", two=2), [], ['biT'])
                em.load('sp', Cn_re[:], s_c_re[j].rearrange("(blk g) i p -> (g i) blk p", g=8), [], ['Cn_re'])
                em.load('sp', Cn_im[:], s_c_im[j].rearrange("(blk g) i p -> (g i) blk p", g=8), [], ['Cn_im'])
                em.load('sp', dsb[:], s_d[j, :].partition_broadcast(128), [], ['dsb'])
                em.act(ldtT[:], ldtT[:], AF.Exp, ['ldtT'], ['ldtT'])
                em.tt('dve', thT[:], limT[:], ldtT[:], ALU.mult, ['limT', 'ldtT'], ['thT'])
                em.tt('dve', q1[:], lrT[:], ldtT[:], ALU.mult, ['lrT', 'ldtT'], ['q1'])
                em.act(magT[:], q1[:], AF.Exp, ['q1'], ['magT'])
                em.op('pool', lambda e: e.iota(iot[:, 0:N], [[1, N]], base=1, channel_multiplier=0), ['bstart', 'kofs', 'iota32', 'sel', 'maskadd_dummy'], ['iot'])
                em.cp('dve', jrow[:], iot[:, 0:N], ['iot'], ['jrow'])

                def reduce_sin(dst, dname, shift):
                    em.ts('dve', kk[:], phi[:], 1.0 / TWO_PI, ALU.mult, ['phi'], ['kk'], s2=shift / TWO_PI, op1=ALU.add)
                    em.cp('dve', kk[:].bitcast(I32), kk[:], ['kk'], ['kk'])
                    em.cp('dve', kk[:], kk[:].bitcast(I32), ['kk'], ['kk'])
                    em.stt(rr_[:], kk[:], -C1, phi[:], ALU.mult, ALU.add, ['kk', 'phi'], ['rr_'])
                    em.stt(rr_[:], kk[:], -C2, rr_[:], ALU.mult, ALU.add, ['kk', 'rr_'], ['rr_'])
                    if shift != 0.0:
                        em.ts('dve', rr_[:], rr_[:], shift, ALU.add, ['rr_'], ['rr_'])
                    for _ in range(2):
                        em.ts('dve', kk[:], rr_[:], math.pi, ALU.is_gt, ['rr_'], ['kk'])
                        em.stt(rr_[:], kk[:], -TWO_PI, rr_[:], ALU.mult, ALU.add, ['kk', 'rr_'], ['rr_'])
                        em.ts('dve', kk[:], rr_[:], -math.pi, ALU.is_lt, ['rr_'], ['kk'])
                        em.stt(rr_[:], kk[:], TWO_PI, rr_[:], ALU.mult, ALU.add, ['kk', 'rr_'], ['rr_'])
                    em.ts('dve', rr_[:], rr_[:], 3.1415925, ALU.min, ['rr_'], ['rr_'], s2=-3.1415925, op1=ALU.max)
                    em.act(dst, rr_[:], AF.Sin, ['rr_'], [dname])
                for hf in range(2):
                    em.tt('dve', phi[:], thT[:, hf * 16:(hf + 1) * 16].unsqueeze(2).broadcast_to([128, 16, N]), jrow[:].unsqueeze(1).broadcast_to([128, 16, N]), ALU.mult, ['thT', 'jrow'], ['phi'])
                    reduce_sin(sinT[:, hf * 16:(hf + 1) * 16, :], 'sinT', 0.0)
                    reduce_sin(cosT[:, hf * 16:(hf + 1) * 16, :], 'cosT', math.pi / 2)
                em.tt('dve', ab_re[:], magT[:], cosT[:, :, 0], ALU.mult, ['magT', 'cosT'], ['ab_re'])
                em.tt('dve', ab_im[:], magT[:], sinT[:, :, 0], ALU.mult, ['magT', 'sinT'], ['ab_im'])
                em.ts('dve', ab_re[:], ab_re[:], -1.0, ALU.add, ['ab_re'], ['ab_re'])
                em.tt('dve', q1[:], lrT[:], lrT[:], ALU.mult, ['lrT'], ['q1'])
                em.tt('dve', q2[:], limT[:], limT[:], ALU.mult, ['limT'], ['q2'])
                em.tt('dve', q1[:], q1[:], q2[:], ALU.add, ['q1', 'q2'], ['q1'])
                em.op('dve', lambda e: e.reciprocal(out=q1[:], in_=q1[:]), ['q1'], ['q1'])
                em.tt('dve', tre[:], ab_re[:], lrT[:], ALU.mult, ['ab_re', 'lrT'], ['tre'])
                em.tt('dve', q2[:], ab_im[:], limT[:], ALU.mult, ['ab_im', 'limT'], ['q2'])
                em.tt('dve', tre[:], tre[:], q2[:], ALU.add, ['tre', 'q2'], ['tre'])
                em.tt('dve', tre[:], tre[:], q1[:], ALU.mult, ['tre', 'q1'], ['tre'])
                em.tt('dve', tim[:], ab_im[:], lrT[:], ALU.mult, ['ab_im', 'lrT'], ['tim'])
                em.tt('dve', q2[:], ab_re[:], limT[:], ALU.mult, ['ab_re', 'limT'], ['q2'])
                em.tt('dve', tim[:], tim[:], q2[:], ALU.subtract, ['tim', 'q2'], ['tim'])
                em.tt('dve', tim[:], tim[:], q1[:], ALU.mult, ['tim', 'q1'], ['tim'])
                treb = tre[:].unsqueeze(2).broadcast_to([128, 32, 16]); timb = tim[:].unsqueeze(2).broadcast_to([128, 32, 16])
                em.tt('dve', bbr[:], brT[:], treb, ALU.mult, ['brT', 'tre'], ['bbr'])
                em.tt('dve', bt[:], biT[:], timb, ALU.mult, ['biT', 'tim'], ['bt'])
                em.tt('dve', bbr[:], bbr[:], bt[:], ALU.subtract, ['bbr', 'bt'], ['bbr'])
                em.tt('dve', bbi[:], biT[:], treb, ALU.mult, ['biT', 'tre'], ['bbi'])
                em.tt('dve', bt[:], brT[:], timb, ALU.mult, ['brT', 'tim'], ['bt'])
                em.tt('dve', bbi[:], bbi[:], bt[:], ALU.add, ['bbi', 'bt'], ['bbi'])
                for (X_, xn_, bb_, bn_, BB_, BBn) in ((Xre, 'Xre', bbr, 'bbr', BBre, 'BBre'), (Xim, 'Xim', bbi, 'bbi', BBim, 'BBim')):
                    for par in range(2):
                        em.memset('dve', X_[:], 0.0, [xn_])
                        for gl in range(2):
                            dstv = X_[gl * 64:(gl + 1) * 64, :, :].rearrange("p b (r2 par c) -> p b r2 par c", r2=2, par=2)[:, :, :, par, gl * 16:(gl + 1) * 16]
                            srcv = bb_[gl * 64:(gl + 1) * 64, :, :].rearrange("p (b r2 par) i -> p b r2 par i", r2=2, par=2)[:, :, :, par, :]
                            em.cp('dve', dstv, srcv, [bn_, xn_], [xn_])
                        pTb = pb[1][:].bitcast(BF16)
                        for blk in range(8):
                            em.tr(pTb[:, blk * 128:(blk + 1) * 128], X_[:, blk, :], ident_b[:], [xn_, 'ident_b'], ['pb1'])
                        em.cp('act', BB_[:, :, par, :], pTb.rearrange("p (a b) -> p a b", a=8), ['pb1'], [BBn])
                em.op('pool', lambda e: e.iota(mki[:], [[0, 1]], base=0, channel_multiplier=1), [], ['mki'])
                em.op('dve', lambda e: e.tensor_scalar(out=mki[:], in0=mki[:], scalar1=4, scalar2=1, op0=ALU.arith_shift_right, op1=ALU.bitwise_and), ['mki'], ['mki'])
                em.cp('dve', m1[:], mki[:], ['mki'], ['m1'])
                em.ts('dve', m0[:], m1[:], -1.0, ALU.mult, ['m1'], ['m0'], s2=1.0, op1=ALU.add)
                for (Cn_, cn_, Y_, yn_, CC_, ccn, sgn) in ((Cn_re, 'Cn_re', Yr, 'Yr', CCre, 'CCre', 1.0), (Cn_im, 'Cn_im', Yi, 'Yi', CCimN, 'CCimN', -1.0)):
                    em.ts('dve', Y_[:, :, 0:64], Cn_[:], m0[:, 0:1], ALU.mult, [cn_, 'm0'], [yn_])
                    em.ts('dve', Y_[:, :, 64:128], Cn_[:], m1[:, 0:1], ALU.mult, [cn_, 'm1'], [yn_])
                    for blk in range(8):
                        em.tr(pb[2][:, 0:128], Y_[:, blk, :], ident_f[:], [yn_, 'ident_f'], ['pb2'])
                        em.act(CC_[:, 4 * blk:4 * blk + 4, :].rearrange("p a b -> p (a b)"), pb[2][:, 0:128], AF.Copy, ['pb2'], [ccn], scale=sgn)
                em.memset('dve', xprev[:], 0.0, ['xprev'])
                em.fence()
            Win = sa("Win", [128, 8, 1024], BF16); Wglu = sa("Wglu", [128, 8, 2048], BF16)
            uTb = sa("uTb", [128, 8, 128], BF16)
            bre = sa("bre", [128, 4 * N]); bim = sa("bim", [128, 4 * N]); t1 = sa("t1", [128, 4 * N]); t2 = sa("t2", [128, 4 * N])
            dre = sa("dre", [128, 4 * N]); dim_ = sa("dim_", [128, 4 * N]); wre = sa("wre", [128, 4 * N]); wim = sa("wim", [128, 4 * N])
            em.load('pool', Win[:], s_w_in[j].rearrange("(kc p) n -> p kc n", p=128), [], ['Win'])
            em.load('pool', Wglu[:, :, 0:1024], s_w_glu[j].rearrange("(kc p) n -> p kc n", p=128)[:, :, 0:1024], [], ['Wglu'])
            em.load('pool', Wglu[:, :, 1024:2048], s_w_glu[j].rearrange("(kc p) n -> p kc n", p=128)[:, :, 1024:2048], [], ['Wglu'])
            load_layer_common(l)
            for ti in range(NT):
                xb_, xn = front(ti, x_in, first)
                modulate_T(xb_, xn)
                for cc in range(8):
                    bk = 1 + cc // 4
                    for kc in range(8):
                        em.mm(pb[bk][:, (cc % 4) * 128:(cc % 4 + 1) * 128], Win[:, kc, cc * 128:(cc + 1) * 128], hT[:, kc, :], kc == 0, kc == 7, ['Win', 'hT'], ['pb%d' % bk])
                em.cp('act', uTb[:, 0:4, :].rearrange("p a b -> p (a b)"), pb[1][:, :], ['pb1'], ['uTb'])
                em.cp('act', uTb[:, 4:8, :].rearrange("p a b -> p (a b)"), pb[2][:, :], ['pb2'], ['uTb'])
                for n in range(2):
                    for kc in range(8):
                        em.mm(pb[3 + n][:, :], hT[:, kc, :], Win[:, kc, n * 512:(n + 1) * 512], kc == 0, kc == 7, ['hT', 'Win'], ['pb%d' % (3 + n)])
                    em.tt('dve', tmpA[:, n * 512:(n + 1) * 512], pb[3 + n][:, :], dsb[:, n * 512:(n + 1) * 512], ALU.mult, ['pb%d' % (3 + n), 'dsb'], ['tmpA'])
                for g in range(8):
                    bA, bB = (6, 7) if g % 2 == 0 else (1, 2)
                    nA = 'pb%d' % bA; nB = 'pb%d' % bB
                    for r in range(4):
                        rb = 64 * (r // 2)
                        bk_ = bA if r < 2 else bB
                        nk_ = nA if r < 2 else nB
                        em.mm(pb[bk_][:, (r % 2) * 128:(r % 2 + 1) * 128], BBre[rb:rb + 64, g, r % 2, :], uTb[rb:rb + 64, g, :], True, True, ['BBre', 'uTb'], [nk_])
                        em.mm(pb[bk_][:, 256 + (r % 2) * 128:256 + (r % 2 + 1) * 128], BBim[rb:rb + 64, g, r % 2, :], uTb[rb:rb + 64, g, :], True, True, ['BBim', 'uTb'], [nk_])
                    em.cp('act', bre[:, 0:256], pb[bA][:, 0:256], [nA], ['bre'])
                    em.cp('act', bim[:, 0:256], pb[bA][:, 256:512], [nA], ['bim'])
                    em.cp('act', bre[:, 256:512], pb[bB][:, 0:256], [nB], ['bre'])
                    em.cp('act', bim[:, 256:512], pb[bB][:, 256:512], [nB], ['bim'])
                    cs_ = cosT[:, 4 * g:4 * g + 4, :].rearrange("p a b -> p (a b)"); sn_ = sinT[:, 4 * g:4 * g + 4, :].rearrange("p a b -> p (a b)")
                    em.tt('pool', t1[:], cs_, bre[:], ALU.mult, ['cosT', 'bre'], ['t1'])
                    em.tt('pool', t2[:], sn_, bim[:], ALU.mult, ['sinT', 'bim'], ['t2'])
                    em.tt('pool', dre[:], t1[:], t2[:], ALU.add, ['t1', 't2'], ['dre'])
                    em.tt('pool', t1[:], cs_, bim[:], ALU.mult, ['cosT', 'bim'], ['t1'])
                    em.tt('pool', t2[:], sn_, bre[:], ALU.mult, ['sinT', 'bre'], ['t2'])
                    em.tt('pool', dim_[:], t1[:], t2[:], ALU.subtract, ['t1', 't2'], ['dim_'])
                    for r in range(4):
                        st = 4 * g + r
                        mgb = magT[:, st:st + 1].broadcast_to([128, N])
                        em.op('dve', lambda e, mgb=mgb, st=st, r=r: e.tensor_tensor_scan(out=wre[:, r * N:(r + 1) * N], data0=mgb, data1=dre[:, r * N:(r + 1) * N],
                                                                                     initial=xprev[:, 0, st:st + 1], op0=ALU.mult, op1=ALU.add), ['magT', 'dre', 'xprev'], ['wre'])
                        em.op('dve', lambda e, mgb=mgb, st=st, r=r: e.tensor_tensor_scan(out=wim[:, r * N:(r + 1) * N], data0=mgb, data1=dim_[:, r * N:(r + 1) * N],
                                                                                     initial=xprev[:, 1, st:st + 1], op0=ALU.mult, op1=ALU.add), ['magT', 'dim_', 'xprev'], ['wim'])
                    em.tt('dve', dre[:], cs_, wre[:], ALU.mult, ['cosT', 'wre'], ['dre'])
                    em.tt('dve', dim_[:], sn_, wim[:], ALU.mult, ['sinT', 'wim'], ['dim_'])
                    em.tt('dve', bre[:], dre[:], dim_[:], ALU.subtract, ['dre', 'dim_'], ['bre'])
                    em.tt('dve', dre[:], cs_, wim[:], ALU.mult, ['cosT', 'wim'], ['dre'])
                    em.tt('dve', dim_[:], sn_, wre[:], ALU.mult, ['sinT', 'wre'], ['dim_'])
                    em.tt('dve', bim[:], dre[:], dim_[:], ALU.add, ['dre', 'dim_'], ['bim'])
                    em.cp('act', xprev[:, 0, 4 * g:4 * g + 4], bre[:].rearrange("p (a b) -> p a b", a=4)[:, :, N - 1], ['bre'], ['xprev'])
                    em.cp('act', xprev[:, 1, 4 * g:4 * g + 4], bim[:].rearrange("p (a b) -> p a b", a=4)[:, :, N - 1], ['bim'], ['xprev'])
                    for r in range(4):
                        st = 4 * g + r
                        yb = 3 + st // 16
                        yo = pb[yb][:, (st % 16) * 32:(st % 16 + 1) * 32]
                        em.mm(yo, bre[:, r * N:(r + 1) * N], CCre[:, st, :], True, False, ['CCre', 'bre'], ['pb%d' % yb])
                        em.mm(yo, bim[:, r * N:(r + 1) * N], CCimN[:, st, :], False, True, ['CCimN', 'bim'], ['pb%d' % yb])
                for n in range(2):
                    em.tt('dve', r1[:, n * 512:(n + 1) * 512], pb[3 + n][:, :], tmpA[:, n * 512:(n + 1) * 512], ALU.add, ['pb%d' % (3 + n), 'tmpA'], ['r1'])
                em.tt('pool', tmpB[:], r1[:], r1[:], ALU.mult, ['r1'], ['tmpB'])
                em.ts('dve', tmpB[:], tmpB[:], 0.044715, ALU.mult, ['tmpB'], ['tmpB'], s2=1.0, op1=ALU.add)
                em.tt('dve', tmpB[:], tmpB[:], r1[:], ALU.mult, ['tmpB', 'r1'], ['tmpB'])
                em.act(tmpB[:], tmpB[:], AF.Sigmoid, ['tmpB'], ['tmpB'], scale=1.5957691216057308)
                em.tt('dve', ub[:], tmpB[:], r1[:], ALU.mult, ['tmpB', 'r1'], ['ub'])
                pT = pb[0][:].bitcast(BF16)
                for kc in range(8):
                    em.tr(pT[:, kc * 128:(kc + 1) * 128], ub[:, kc * 128:(kc + 1) * 128], ident_b[:], ['ub', 'ident_b'], ['pb0'])
                em.cp('act', uT[:].rearrange("p a b -> p (a b)"), pT, ['pb0'], ['uT'])
                for n4 in (2, 3, 0, 1):
                    bk = 3 + n4 % 2
                    for cc in range(8):
                        em.mm(pb[bk][:, :], uT[:, cc, :], Wglu[:, cc, n4 * 512:(n4 + 1) * 512], cc == 0, cc == 7, ['uT', 'Wglu'], ['pb%d' % bk])
                    if n4 >= 2:
                        em.act(tmpB[:, (n4 - 2) * 512:(n4 - 1) * 512], pb[bk][:, :], AF.Sigmoid, ['pb%d' % bk], ['tmpB'])
                    else:
                        em.tt('dve', tmpA[:, n4 * 512:(n4 + 1) * 512], pb[bk][:, :], tmpB[:, n4 * 512:(n4 + 1) * 512], ALU.mult, ['pb%d' % bk, 'tmpB'], ['tmpA'])
                em.tt('pool', tmpA[:], tmpA[:], g1b[:], ALU.mult, ['tmpA', 'g1b'], ['tmpA'])
                after_mix(ti, xb_, xn)
            load_layer_tail(l)
            em.fence()

    def swa_layer(l, first):
        j = l // 3
        with ExitStack() as ar:
            def sa(name, shape, dt=F32):
                return ar.enter_context(nc.sbuf_tensor(name + '_u%d' % next(uid), list(shape), dt))
            Wq = sa("Wq", [128, 8, 1024], BF16); Wkd = sa("Wkd", [128, 8, 4, 128], BF16); Wv = sa("Wv", [128, 8, 256], BF16)
            Wo = sa("Wo", [128, 8, 1024], BF16)
            bq = sa("bq", [128, 8]); bkd = sa("bkd", [128, 4]); bvb = sa("bvb", [128, 256]); sinkb = sa("sinkb", [128, 16])
            maskadd = sa("maskadd", [128, 256])
            qT = sa("qT", [128, 8, 128], BF16)
            kT2 = [sa("kT2_%d" % i, [128, 4, 128], BF16) for i in range(2)]
            Vt = [sa("Vt%d" % i, [128, 256], BF16) for i in range(2)]
            sc_ = [sa("sc%d" % i, [128, 256]) for i in range(2)]; pexp_ = [sa("pexp%d" % i, [128, 256], BF16) for i in range(2)]
            ppT_ = [sa("ppT%d" % i, [128, 2, 128], BF16) for i in range(2)]
            sv_ = [sa("sv%d" % i, [128, 8]) for i in range(2)]
            wv_ = a_w_qkv[j].rearrange("(kc p) n -> p kc n", p=128)
            em.load('pool', Wq[:], wv_[:, :, 0:1024], [], ['Wq'])
            for half in range(2):
                for h in range(4):
                    em.load('pool', Wkd[:, :, h, half * 64:(half + 1) * 64], wv_[:, :, 1024 + h * 64:1088 + h * 64], [], ['Wkd'])
            em.load('pool', Wv[:], wv_[:, :, 1280:1536], [], ['Wv'])
            em.load('pool', Wo[:], a_w_o[j].rearrange("(kc p) n -> p kc n", p=128), [], ['Wo'])
            em.load('sp', bq[:], a_b_qkv[j, 0:1024].rearrange("(cc p) -> p cc", p=128), [], ['bq'])
            for half in range(2):
                em.load('sp', bkd[half * 64:(half + 1) * 64, :], a_b_qkv[j, 1024:1280].rearrange("(h p) -> p h", p=64), [], ['bkd'])
            em.load('sp', bvb[:], a_b_qkv[j, 1280:1536].partition_broadcast(128), [], ['bvb'])
            em.load('sp', sinkb[:], a_sinks[j, :].partition_broadcast(128), [], ['sinkb'])
            em.op('pool', lambda e: e.iota(iot[:, 0:128], [[1, 128]], base=0, channel_multiplier=-1), ['bstart', 'kofs', 'iota32', 'sel'], ['iot'])
            em.ts('dve', maskadd[:, 0:128], iot[:, 0:128], 0.0, ALU.is_le, ['iot'], ['maskadd'], s2=-1e5, op1=ALU.mult)
            em.ts('dve', maskadd[:, 128:256], iot[:, 0:128], 0.0, ALU.is_gt, ['iot'], ['maskadd'], s2=-1e5, op1=ALU.mult)
            load_layer_common(l)
            for ti in range(NT):
                xb_, xn = front(ti, x_in, first)
                modulate_T(xb_, xn)
                cur = ti % 2; prv = (ti + 1) % 2
                kc_n = 'kT2_%d' % cur; kp_n = 'kT2_%d' % prv; vc_n = 'Vt%d' % cur; vp_n = 'Vt%d' % prv
                for cc in range(8):
                    bk = 1 + cc // 4
                    for kc in range(8):
                        em.mm(pb[bk][:, (cc % 4) * 128:(cc % 4 + 1) * 128], Wq[:, kc, cc * 128:(cc + 1) * 128], hT[:, kc, :], kc == 0, kc == 7, ['Wq', 'hT'], ['pb%d' % bk])
                    em.act(qT[:, cc, :], pb[bk][:, (cc % 4) * 128:(cc % 4 + 1) * 128], AF.Identity, ['pb%d' % bk, 'bq'], ['qT'], bias=bq[:, cc:cc + 1])
                for h in range(4):
                    for kc in range(8):
                        em.mm(pb[3][:, h * 128:(h + 1) * 128], Wkd[:, kc, h, :], hT[:, kc, :], kc == 0, kc == 7, ['Wkd', 'hT'], ['pb3'])
                    em.act(kT2[cur][:, h, :], pb[3][:, h * 128:(h + 1) * 128], AF.Identity, ['pb3', 'bkd'], [kc_n], bias=bkd[:, h:h + 1])
                for kc in range(8):
                    em.mm(pb[4][:, 0:256], hT[:, kc, :], Wv[:, kc, :], kc == 0, kc == 7, ['hT', 'Wv'], ['pb4'])
                em.tt('dve', Vt[cur][:], pb[4][:, 0:256], bvb[:], ALU.add, ['pb4', 'bvb'], [vc_n])
                c0 = 0 if ti > 0 else 128
                def swa_gen(hq):
                    hkv = hq // 4; pbase = (hq % 2) * 64
                    ql = qT[pbase:pbase + 64, hq // 2, :]
                    so = 0
                    hp = hq % 2; sx = str(hp)
                    sc = sc_[hp]; pexp = pexp_[hp]; ppT = ppT_[hp]; sv = sv_[hp]
                    sbk = 6 if hp == 0 else 2
                    sbn = 'pb%d' % sbk
                    if ti > 0:
                        em.mm(pb[sbk][:, so:so + 128], ql, kT2[prv][pbase:pbase + 64, hkv, :], True, True, ['qT', kp_n], [sbn])
                    em.mm(pb[sbk][:, so + 128:so + 256], ql, kT2[cur][pbase:pbase + 64, hkv, :], True, True, ['qT', kc_n], [sbn])
                    yield
                    em.tt('dve', sc[:, c0:256], pb[sbk][:, so + c0:so + 256], maskadd[:, c0:256], ALU.add, [sbn, 'maskadd'], ['sc' + sx])
                    em.red(sv[:, 0:1], sc[:, c0:256], ALU.max, ['sc' + sx], ['sv' + sx])
                    em.ts('dve', sv[:, 1:2], sv[:, 0:1], 0.125, ALU.mult, ['sv' + sx, 'sinkb'], ['sv' + sx], s2=sinkb[:, hq:hq + 1], op1=ALU.max)
                    em.ts('dve', sv[:, 2:3], sv[:, 1:2], -1.0, ALU.mult, ['sv' + sx], ['sv' + sx])
                    yield
                    em.act(pexp[:, c0:256], sc[:, c0:256], AF.Exp, ['sc' + sx, 'sv' + sx], ['pexp' + sx, 'sv' + sx], bias=sv[:, 2:3], scale=0.125, accum=sv[:, 3:4])
                    em.act(sv[:, 4:5], sv[:, 2:3], AF.Exp, ['sv' + sx, 'sinkb'], ['sv' + sx], bias=sinkb[:, hq:hq + 1])
                    yield
                    em.tt('dve', sv[:, 5:6], sv[:, 3:4], sv[:, 4:5], ALU.add, ['sv' + sx], ['sv' + sx])
                    em.op('dve', lambda e, sv=sv: e.reciprocal(out=sv[:, 6:7], in_=sv[:, 5:6]), ['sv' + sx], ['sv' + sx])
                    yield
                    tbk = 5 if hp == 0 else 1
                    pT5 = pb[tbk][:].bitcast(BF16)
                    blks = [0, 1] if ti > 0 else [1]
                    for bi in blks:
                        em.tr(pT5[:, bi * 128:(bi + 1) * 128], pexp[:, bi * 128:(bi + 1) * 128], ident_b[:], ['pexp' + sx, 'ident_b'], ['pb%d' % tbk])
                    yield
                    em.cp('act', ppT[:, blks[0]:2, :].rearrange("p a b -> p (a b)"), pT5[:, blks[0] * 128:256], ['pb%d' % tbk], ['ppT' + sx])
                    yield
                    obk = 7 if hp == 0 else 0
                    oc = pb[obk][:, (hq // 2) * 64:(hq // 2 + 1) * 64]
                    for ii_, bi in enumerate(blks):
                        vsrc = Vt[prv] if bi == 0 else Vt[cur]
                        em.mm(oc, ppT[:, bi, :], vsrc[:, hkv * 64:(hkv + 1) * 64], ii_ == 0, ii_ == len(blks) - 1, ['ppT' + sx, vp_n if bi == 0 else vc_n], ['pb%d' % obk])
                    yield
                    em.ts('dve', ub[:, hq * 64:(hq + 1) * 64], oc, sv[:, 6:7], ALU.mult, ['pb%d' % obk, 'sv' + sx], ['ub'])

                    yield
                for pair in range(8):
                    gens = [swa_gen(2 * pair), swa_gen(2 * pair + 1)]
                    alive = True
                    while alive:
                        alive = False
                        for g_ in gens:
                            try:
                                next(g_)
                                alive = True
                            except StopIteration:
                                pass
                post(ti, xb_, xn, Wo, 'Wo')
            load_layer_tail(l)
            em.fence()

    adaln_phase()
    first = True
    for li_, l in enumerate(layers):
        kind = l % 3
        if kind == 0:
            mlstm_layer(l, first)
        elif kind == 2:
            swa_layer(l, first)
        else:
            s5_layer(l, first)
        moe(l)
        first = False
    for ti in range(NT):
        xb_ = xt[0]; xn = 'xt0'
        combine_tile(ti, xb_, xn)
        em.load('sp', out_d[ti * 128:(ti + 1) * 128, :], xb_[:], [xn], ['out_d'])
    em.finish()
    with nc.allow_non_contiguous_dma(reason="small strided parameter loads"):
        with nc.Block() as block:
            em.emit(block)
    es.close()
    return nc


INPUT_NAMES = ["ada_w", "ada_b", "ln1_g", "ln1_b", "ln2_g", "ln2_b", "mlstm_w_in", "mlstm_conv_w", "mlstm_conv_b", "mlstm_b_if",
               "mlstm_norm_g", "mlstm_w_out", "s5_w_in", "s5_lam_re", "s5_lam_im", "s5_log_dt", "s5_b_re", "s5_b_im", "s5_c_re",
               "s5_c_im", "s5_d", "s5_w_glu", "swa_w_qkv", "swa_b_qkv", "swa_sinks", "swa_w_o", "moe_w_group", "moe_b_group",
               "moe_w_router", "moe_b_router", "moe_w1", "moe_w3", "moe_w2"]


def run(inputs, S, layers, ncores, dbg=False, trace=False):
    nc = build(S, layers, ncores, dbg)
    in_maps = []
    for c in range(ncores):
        m = {"x": np.ascontiguousarray(inputs["x"][c, :S]), "c": np.ascontiguousarray(inputs["c"][c:c + 1])}
        for n in INPUT_NAMES:
            a = np.asarray(inputs[n])
            if n in ("moe_w1", "moe_w3", "moe_w2"):
                a = a.reshape(DEPTH * NE * 256, 2048)
            m[n] = a
        in_maps.append(m)
    res = run_bass_kernel_spmd(nc, in_maps, core_ids=list(range(ncores)), trace=trace)
    return res


def kernel(**inputs):
    res = run(inputs, 16384, [0, 1, 2, 3], 2)
    return np.stack([res.results[c]["out"] for c in range(2)], axis=0)
```
